# Optimizing a Trainium2 kernel written in Bass

```python
import jax, jax.numpy as jnp
from jax import lax

D_MODEL = 1024
BATCH = 16
SEQ = 2048
DEPTH = 2

D_MIX = D_MODEL
CHUNK = 128
A_GROUPS = 4
A_WIDTH = D_MIX // 4
A_HEAD = A_WIDTH // A_GROUPS
B_GROUPS = 4
B_WIDTH = D_MIX // 4
CONV_W = 3
C_HEADS = 4
C_WIDTH = D_MIX // 2
C_HEAD = C_WIDTH // C_HEADS
ROPE_BASE = 10000.0
D_PROJ = 2 * A_WIDTH + 3 * B_WIDTH + 4 * C_WIDTH
D_FF = 2816
MACARON = 0.5
EPS = 1e-6

kernel_name = "hybrid_gmlp_shortconv_retention_macaron"


def _rmsnorm(x, g):
    xf = x.astype(jnp.float32)
    y = xf * lax.rsqrt(jnp.mean(xf * xf, axis=-1, keepdims=True) + EPS)
    return (y * g.astype(jnp.float32)).astype(x.dtype)


def _group_layernorm(x, g, groups):
    shp = x.shape
    xf = x.astype(jnp.float32).reshape(shp[:-1] + (groups, shp[-1] // groups))
    mu = jnp.mean(xf, axis=-1, keepdims=True)
    var = jnp.mean(jnp.square(xf - mu), axis=-1, keepdims=True)
    y = ((xf - mu) * lax.rsqrt(var + EPS)).reshape(shp)
    return (y * g.astype(jnp.float32)).astype(x.dtype)


def _swiglu(x, w_gate, w_up, w_down):
    return (jax.nn.silu(x @ w_gate) * (x @ w_up)) @ w_down


def _gmlp_mixer(p, v_norm, w_s, b_s):
    bsz, s, _ = p.shape
    nc = s // CHUNK
    z = jax.nn.gelu(p)
    u, v = z[..., :A_WIDTH], z[..., A_WIDTH:]
    v = _group_layernorm(v, v_norm, A_GROUPS)
    v = v.reshape(bsz, nc, CHUNK, A_GROUPS, A_HEAD)
    causal = jnp.tril(jnp.ones((CHUNK, CHUNK), dtype=w_s.dtype))
    ws = w_s * causal[None]
    sv = jnp.einsum('gts,bnsgd->bntgd', ws, v) + b_s.T[None, None, :, :, None]
    return u * sv.reshape(bsz, s, A_WIDTH)


def _shortconv_mixer(p, conv_w):
    s = p.shape[1]
    bg, cg, xin = p[..., :B_WIDTH], p[..., B_WIDTH:2 * B_WIDTH], p[..., 2 * B_WIDTH:]
    z = cg * xin
    zp = jnp.pad(z, ((0, 0), (CONV_W - 1, 0), (0, 0)))
    y = sum(zp[:, i:i + s, :] * conv_w[i] for i in range(CONV_W))
    return bg * y


def _rotary(x, pos):
    half = x.shape[-1] // 2
    inv = ROPE_BASE ** (-jnp.arange(half, dtype=jnp.float32) / half)
    ang = pos[:, None] * inv[None, :]
    cos = jnp.cos(ang)[None, :, None, :]
    sin = jnp.sin(ang)[None, :, None, :]
    x1, x2 = x[..., :half], x[..., half:]
    return jnp.concatenate([x1 * cos - x2 * sin, x2 * cos + x1 * sin], axis=-1)


def _retention_mixer(p, gn):
    dtype = p.dtype
    bsz, s, _ = p.shape
    nc = s // CHUNK
    pf = p.astype(jnp.float32)
    q = pf[..., 0 * C_WIDTH:1 * C_WIDTH].reshape(bsz, s, C_HEADS, C_HEAD)
    k = pf[..., 1 * C_WIDTH:2 * C_WIDTH].reshape(bsz, s, C_HEADS, C_HEAD)
    v = pf[..., 2 * C_WIDTH:3 * C_WIDTH].reshape(bsz, s, C_HEADS, C_HEAD)
    g = p[..., 3 * C_WIDTH:]
    pos = jnp.arange(s, dtype=jnp.float32)
    q = _rotary(q, pos)
    k = _rotary(k, pos) * (C_HEAD ** -0.5)

    log_g = jnp.log1p(-jnp.exp2(-5.0 - jnp.arange(C_HEADS, dtype=jnp.float32)))
    idx = jnp.arange(CHUNK, dtype=jnp.float32)
    diff = idx[:, None] - idx[None, :]
    decay = jnp.where(diff[None] >= 0,
                      jnp.exp(log_g[:, None, None] * jnp.maximum(diff, 0.0)[None]), 0.0)
    zeta = jnp.exp(log_g[None, :] * (CHUNK - 1 - idx)[:, None])
    xi = jnp.exp(log_g[None, :] * (idx + 1.0)[:, None])
    gamma_chunk = jnp.exp(log_g * CHUNK)

    qc = q.reshape(bsz, nc, CHUNK, C_HEADS, C_HEAD)
    kc = k.reshape(bsz, nc, CHUNK, C_HEADS, C_HEAD)
    vc = v.reshape(bsz, nc, CHUNK, C_HEADS, C_HEAD)

    scores = jnp.einsum('bnthd,bnshd->bnhts', qc, kc) * decay[None, None]
    inner = jnp.einsum('bnhts,bnshv->bnthv', scores, vc)

    chunk_kv = jnp.einsum('bnshd,sh,bnshv->bnhdv', kc, zeta, vc)

    def step(state, kv):
        return state * gamma_chunk[None, :, None, None] + kv, state

    init = jnp.zeros((bsz, C_HEADS, C_HEAD, C_HEAD), jnp.float32)
    _, prev = lax.scan(step, init, jnp.moveaxis(chunk_kv, 1, 0))
    cross = jnp.einsum('bnthd,nbhdv,th->bnthv', qc, prev, xi)

    ret = (inner + cross).reshape(bsz, s, C_WIDTH)
    ret = _group_layernorm(ret, gn, C_HEADS).astype(dtype)
    return jax.nn.silu(g) * ret


def _layer(x, ffn1_norm, ffn1_w_gate, ffn1_w_up, ffn1_w_down, mix_norm, w_in,
           gmlp_v_norm, gmlp_w_s, gmlp_b_s, conv_w, ret_gn, w_out,
           ffn2_norm, ffn2_w_gate, ffn2_w_up, ffn2_w_down):
    x = x + MACARON * _swiglu(_rmsnorm(x, ffn1_norm), ffn1_w_gate, ffn1_w_up, ffn1_w_down)
    h = _rmsnorm(x, mix_norm)
    p = h @ w_in
    a_end = 2 * A_WIDTH
    b_end = a_end + 3 * B_WIDTH
    y_a = _gmlp_mixer(p[..., :a_end], gmlp_v_norm, gmlp_w_s, gmlp_b_s)
    y_b = _shortconv_mixer(p[..., a_end:b_end], conv_w)
    y_c = _retention_mixer(p[..., b_end:], ret_gn)
    x = x + jnp.concatenate([y_a, y_b, y_c], axis=-1) @ w_out
    x = x + MACARON * _swiglu(_rmsnorm(x, ffn2_norm), ffn2_w_gate, ffn2_w_up, ffn2_w_down)
    return x


def setup_inputs(seed: int = 0) -> dict:
    key = jax.random.key(seed)
    ks = jax.random.split(key, 18)
    f32 = jnp.float32

    def w(k, shape, fan_in):
        return jax.random.normal(k, shape, f32) * (fan_in ** -0.5)

    def gain(k, shape):
        return 1.0 + 0.1 * jax.random.normal(k, shape, f32)

    return {
        "x": jax.random.normal(ks[0], (BATCH, SEQ, D_MODEL), f32),
        "ffn1_norm": gain(ks[1], (DEPTH, D_MODEL)),
        "ffn1_w_gate": w(ks[2], (DEPTH, D_MODEL, D_FF), D_MODEL),
        "ffn1_w_up": w(ks[3], (DEPTH, D_MODEL, D_FF), D_MODEL),
        "ffn1_w_down": w(ks[4], (DEPTH, D_FF, D_MODEL), D_FF),
        "mix_norm": gain(ks[5], (DEPTH, D_MODEL)),
        "w_in": w(ks[6], (DEPTH, D_MODEL, D_PROJ), D_MODEL),
        "gmlp_v_norm": gain(ks[7], (DEPTH, A_WIDTH)),
        "gmlp_w_s": w(ks[8], (DEPTH, A_GROUPS, CHUNK, CHUNK), CHUNK),
        "gmlp_b_s": 1.0 + 0.1 * jax.random.normal(ks[9], (DEPTH, A_GROUPS, CHUNK), f32),
        "conv_w": w(ks[10], (DEPTH, CONV_W, B_WIDTH), CONV_W),
        "ret_gn": gain(ks[11], (DEPTH, C_WIDTH)),
        "w_out": w(ks[12], (DEPTH, D_MIX, D_MODEL), D_MIX),
        "ffn2_norm": gain(ks[13], (DEPTH, D_MODEL)),
        "ffn2_w_gate": w(ks[14], (DEPTH, D_MODEL, D_FF), D_MODEL),
        "ffn2_w_up": w(ks[15], (DEPTH, D_MODEL, D_FF), D_MODEL),
        "ffn2_w_down": w(ks[16], (DEPTH, D_FF, D_MODEL), D_FF),
        "final_norm": gain(ks[17], (D_MODEL,)),
    }


def reference(x, ffn1_norm, ffn1_w_gate, ffn1_w_up, ffn1_w_down, mix_norm, w_in,
              gmlp_v_norm, gmlp_w_s, gmlp_b_s, conv_w, ret_gn, w_out,
              ffn2_norm, ffn2_w_gate, ffn2_w_up, ffn2_w_down, final_norm):
    for l in range(DEPTH):
        x = _layer(x, ffn1_norm[l], ffn1_w_gate[l], ffn1_w_up[l], ffn1_w_down[l],
                   mix_norm[l], w_in[l], gmlp_v_norm[l], gmlp_w_s[l], gmlp_b_s[l],
                   conv_w[l], ret_gn[l], w_out[l],
                   ffn2_norm[l], ffn2_w_gate[l], ffn2_w_up[l], ffn2_w_down[l])
    return _rmsnorm(x, final_norm)
```

```python
import math
from contextlib import ExitStack

import numpy as np
import concourse.bass as bass
import concourse.mybir as mybir
from concourse.bass_utils import run_bass_kernel_spmd

F32 = mybir.dt.float32
BF16 = mybir.dt.bfloat16
AF = mybir.ActivationFunctionType
ALU = mybir.AluOpType

D = 1024
SEQ = 2048
NSEQ = 2
L = 2
DFF = 2816
NJ = 22
TILE = 512
NT = SEQ // TILE
CH = 128
NCH = SEQ // CH
GROUPS = [(0, 6), (6, 12), (12, 17), (17, 22)]
EPS = 1e-6
NCV = 68
NCST = 128 + 512 + 128 + 4 + 4 + 4 + 8
SEM_LIMIT = 3000
DBG = {"ffn": 9, "ng": 4}
POOL_KC = ()
OFFLOAD = True


class SemH:
    __slots__ = ("h", "eng", "cnt")

    def __init__(self, h, eng=None):
        self.h = h
        self.eng = eng
        self.cnt = 0


class Buf:
    __slots__ = ("w", "rd", "name")

    def __init__(self, name=""):
        self.w = None
        self.rd = {}
        self.name = name


class Eng:
    def __init__(self, name, sems, is_pe=False):
        self.name = name
        self.sems = sems
        for s in sems:
            s.eng = self
        self.si = 0
        self.ops = []
        self.known = {}
        self.is_pe = is_pe

    def cur(self):
        return self.sems[self.si]


class Builder:
    def __init__(self, nc, es):
        self.nc = nc
        self.es = es
        self.sem_i = 0

    def new_sem(self, name=None):
        self.sem_i += 1
        return SemH(self.es.enter_context(self.nc.semaphore(name or f"s{self.sem_i}")))

    def mk_eng(self, name, nsem, is_pe=False):
        return Eng(name, [self.new_sem(f"{name}{i}") for i in range(nsem)], is_pe)

    def _collect(self, eng, rd, wr):
        waits = {}

        def need(s, v):
            if s.eng is eng and eng.is_pe:
                return
            if eng.known.get(s, 0) >= v:
                return
            if waits.get(s, 0) < v:
                waits[s] = v

        for b in rd:
            if b.w is not None:
                need(*b.w)
        for b in wr:
            if b.w is not None:
                need(*b.w)
            for s, v in b.rd.items():
                need(s, v)
        for s, v in waits.items():
            eng.known[s] = v
        return [(s.h, v) for s, v in waits.items()]

    @staticmethod
    def _record(tag, rd, wr):
        s, v = tag
        for b in rd:
            if b.rd.get(s, 0) < v:
                b.rd[s] = v
        for b in wr:
            b.w = tag
            b.rd = {}

    def op(self, eng, fn, rd=(), wr=(), inc=True):
        waits = self._collect(eng, rd, wr)
        sem = eng.cur()
        if inc:
            sem.cnt += 1
            tag = (sem, sem.cnt)
            eng.ops.append((waits, fn, sem.h, 1))
            if sem.cnt >= SEM_LIMIT:
                eng.si += 1
        else:
            tag = (sem, sem.cnt + 1)
            eng.ops.append((waits, fn, None, 0))
        self._record(tag, rd, wr)

    def dma(self, q, dsem, out, in_, rd=(), wr=()):
        waits = self._collect(q, rd, wr)
        dsem.cnt += 16
        tag = (dsem, dsem.cnt)
        q.ops.append((waits, lambda e: e.dma_start(out=out, in_=in_), dsem.h, 16))
        self._record(tag, rd, wr)


def _replay(e, ops):
    for waits, fn, inc, amt in ops:
        for h, v in waits:
            e.wait_ge(h, v)
        ins = fn(e)
        if inc is not None:
            ins.then_inc(inc, amt)


def alias_switch(old, new):
    tags = {}
    for b in old:
        if b.w is not None:
            s, v = b.w
            if tags.get(s, 0) < v:
                tags[s] = v
        for s, v in b.rd.items():
            if tags.get(s, 0) < v:
                tags[s] = v
    for b in new:
        b.w = None
        b.rd = dict(tags)


def build(n_phases=None, final_norm=True, n_seq=NSEQ):
    nc = bass.Bass("TRN2", target_bir_lowering=False)

    def dram(name, shape, kind="ExternalInput"):
        return nc.dram_tensor(name, shape, F32, kind=kind).ap()

    x_d = dram("x", [NSEQ, D, SEQ])
    wgu_d = dram("wgu", [L * 2 * NJ, 128, 2048])
    wd_d = dram("wd", [L * 2, DFF, D])
    wins_d = dram("wins", [L * 5, 128, 2048])
    winc_d = dram("winc", [L * 4, 128, 4096])
    wout_d = dram("wout", [L * 4, 128, 2048])
    colv_d = dram("colv", [128, NCV])
    rowb_d = dram("rowb", [L, 128, 1024])
    wst_d = dram("wst", [L, 128, 512])
    cst_d = dram("cst", [128, NCST])
    cs_d = dram("cossin", [128, 2048])
    out_d = dram("out", [NSEQ, D, SEQ], kind="ExternalOutput")

    es = ExitStack()
    with es:
        B = Builder(nc, es)
        PE = B.mk_eng("pe", 6, is_pe=True)
        ACT = B.mk_eng("act", 6)
        DVE = B.mk_eng("dve", 8)
        POOL = B.mk_eng("pool", 2)
        SP = B.mk_eng("sp", 2)

        def sb(name, shape, dt):
            return es.enter_context(nc.sbuf_tensor(name, shape, dt))

        xT_t = sb("xT", [128, 8 * SEQ], F32)
        xT = xT_t[:].rearrange("p (k n) -> p k n", k=8)
        cst = sb("cst_sb", [128, NCST], F32)
        ident_f = cst[:, 0:128]
        M2 = cst[:, 128:640].rearrange("p (h t) -> p h t", h=4)
        maskT = cst[:, 640:768]
        zs = cst[:, 768:772]
        epsxi = cst[:, 772:776]
        gch_c = cst[:, 776:780]
        eps8 = cst[:, 780:788]
        colv = sb("colv_sb", [128, NCV], F32)
        ident_b = sb("identb", [128, 128], BF16)
        ones_b = sb("onesb", [128, 128], BF16)

        NR = 3
        SZ_B = 41024
        SZ_C = 63488
        SZ_D = 24576
        ARENA = NR * 4096 + SZ_B + SZ_C + SZ_D
        arena = sb("arena", [128, ARENA // 2], BF16)

        def av(off, n, dt):
            if dt is F32:
                return arena[:, off // 2:(off + 4 * n) // 2].bitcast(F32)
            return arena[:, off // 2:(off + 2 * n) // 2]

        ringA = [av(i * 4096, 2048, BF16) for i in range(NR)]
        oB = NR * 4096
        wd_slots = [av(oB + i * 12288, 6144, BF16).rearrange("p (j n) -> p j n", j=6) for i in range(2)]
        hm2 = [av(oB + i * 8192, 4096, BF16).rearrange("p (k n) -> p k n", k=8) for i in range(2)]
        ycat = [av(oB + 16384 + i * 8192, 4096, BF16).rearrange("p (k n) -> p k n", k=8) for i in range(2)]
        uT = [av(oB + 32768 + i * 2048, 1024, BF16).rearrange("p (k n) -> p k n", k=2) for i in range(2)]
        zc = av(oB + 36864, 1032, F32).rearrange("p (k n) -> p k n", k=2)
        assert 36864 + 4128 <= SZ_B
        oC = oB + SZ_B
        hT = av(oC, 8 * SEQ, BF16).rearrange("p (k n) -> p k n", k=8)
        aT = av(oC + 32768, 6 * SEQ, BF16).rearrange("p (k n) -> p k n", k=6)
        stg = [av(oC + i * 4096, 1024, F32) for i in range(2)]
        winc_flat = [av(oC + i * 8192, 4096, BF16) for i in range(4)]
        winc = [w_.rearrange("p (k n) -> p k n", k=8) for w_ in winc_flat]
        wva_flat = av(oC + 32768, 2048, BF16)
        wva = wva_flat.rearrange("p (k n) -> p k n", k=8)
        oK = oC + 32768 + 4096
        _ko = [oK]

        def kalloc(n, dt):
            o = _ko[0]
            nb = n * (4 if dt is F32 else 2)
            _ko[0] = o + ((nb + 31) // 32) * 32
            return av(o, n, dt)

        vg = kalloc(256, F32)
        sqA = kalloc(256, BF16)
        vz = kalloc(512, BF16).rearrange("p (g n) -> p g n", g=4)
        vtok = [kalloc(512, BF16) for _ in range(2)]
        sgc = [kalloc(512, F32) for _ in range(2)]
        t1 = kalloc(512, F32)
        t2 = kalloc(512, F32)
        qrot = kalloc(512, BF16)
        krot = kalloc(512, BF16)
        kz = [kalloc(512, BF16) for _ in range(2)]
        qkT = kalloc(1024, BF16)
        sT = kalloc(512, BF16).rearrange("p (h n) -> p h n", h=4)
        rn = kalloc(512, F32)
        sqC = kalloc(512, BF16)
        yc = [kalloc(512, BF16) for _ in range(2)]
        svt = kalloc(256, F32)
        s1A = kalloc(4, F32)
        s1C = kalloc(4, F32)
        var8 = [kalloc(8, F32) for _ in range(2)]
        rs8 = [kalloc(8, F32) for _ in range(2)]
        assert _ko[0] <= oC + SZ_C, _ko[0] - oC
        oD = oC + SZ_C
        sq = [av(oD + i * 1024, 512, BF16) for i in range(4)]
        rstd = av(oD + 4096, 512, F32)
        rt = rstd
        cosT = av(oD + 6144, 1024, F32).rearrange("p (c i) -> p c i", c=16)
        sinT = av(oD + 10240, 1024, F32).rearrange("p (c i) -> p c i", c=16)
        vnorm_b = av(oD + 14336, 256, F32)
        gn_b = av(oD + 15360, 512, F32)
        bsb = av(oD + 17408, 256, F32).rearrange("p (k n) -> p k n", k=2)
        oD2 = oD + 18432
        sg = [av(oD2 + i * 2048, 512, F32) for i in range(3)]
        Sst = av(oD2, 512, F32).rearrange("p (h n) -> p h n", h=4)
        Sbf = av(oD2 + 2048, 512, BF16).rearrange("p (h n) -> p h n", h=4)
        wsT_b = av(oD2 + 3072, 512, BF16).rearrange("p (g n) -> p g n", g=4)
        wsT_f = av(oD2 + 4096, 512, F32)
        assert oD + SZ_D == ARENA

        banks = [es.enter_context(nc.psum_tensor(f"ps{i}", [128, 512], F32)) for i in range(8)]
        bank_bufs = [Buf(f"ps{i}") for i in range(8)]
        _bi = [0]

        def ps_next():
            i = _bi[0] % 8
            _bi[0] += 1
            return banks[i][:], bank_bufs[i]

        xb = [[Buf(f"x{k}_{t}") for t in range(NT)] for k in range(8)]
        hb = [[Buf() for t in range(NT)] for k in range(8)]
        ab = [[Buf() for t in range(NT)] for k in range(6)]
        ringA_b = [Buf(f"ringA{i}") for i in range(NR)]
        ringA_s = [B.new_sem(f"dA{i}") for i in range(NR)]
        wd_b = [Buf(), Buf()]
        wd_s = [B.new_sem("dwd0"), B.new_sem("dwd1")]
        winc_b = [Buf() for _ in range(4)]
        winc_s = [B.new_sem(f"dwc{i}") for i in range(4)]
        wva_b = Buf()
        wva_s = B.new_sem("dwva")
        stg_b = [Buf(), Buf()]
        stg_s = [B.new_sem("dst0"), B.new_sem("dst1")]
        sq_b = [Buf() for _ in range(4)]
        rstd_b = Buf()
        rt_b = rstd_b
        sg_b = [Buf() for _ in range(3)]
        cst_b, colv_b, cs_b, rowb_b, wst_b = Buf(), Buf(), Buf(), Buf(), Buf()
        cst_s, rowb_s = B.new_sem("dcst"), B.new_sem("drowb")
        idb_b, ones_bb = Buf(), Buf()
        hm_b2 = [Buf(), Buf()]
        ycA_b = [[Buf() for _ in range(4)] for _ in range(2)]
        ycB_b = [Buf(), Buf()]
        ycC_b = [[Buf() for _ in range(4)] for _ in range(2)]
        uT_b = [Buf(), Buf()]
        zc_b = [Buf(), Buf()]
        vg_b, sqA_b, vz_b, t1_b, t2_b, rn_b, sqC_b, svt_b = (Buf() for _ in range(8))
        vtok_b = [Buf(), Buf()]
        sgc_b = [Buf(), Buf()]
        qrot_b, krot_b, qkT_b, sT_b = Buf(), Buf(), Buf(), Buf()
        kz_b = [Buf(), Buf()]
        yc_b = [Buf(), Buf()]
        s1A_b, s1C_b = Buf(), Buf()
        var8_b = [Buf(), Buf()]
        rs8_b = [Buf(), Buf()]
        S_b, Sbf_b, wsTb_b = Buf(), Buf(), Buf()

        regB_ffn = wd_b
        regB_mix = hm_b2 + ycB_b + uT_b + zc_b + [b for r in ycA_b for b in r] + [b for r in ycC_b for b in r]
        regC_ffn = [b for row in hb for b in row] + [b for row in ab for b in row]
        regC_mix = winc_b + [wva_b, vg_b, sqA_b, vz_b, t1_b, t2_b, rn_b, sqC_b, svt_b, qrot_b, krot_b, qkT_b, sT_b,
                             s1A_b, s1C_b] + vtok_b + sgc_b + kz_b + yc_b + var8_b + rs8_b
        regC_io = stg_b
        regD_ffn = sg_b
        regD_mix = [S_b, Sbf_b, wst_b, wsTb_b]

        phases = []
        for p in range(n_seq):
            for l in range(L):
                phases += [(p, l, "ffn", 0), (p, l, "mix", 0), (p, l, "ffn", 1)]
        run_phases = []
        for p in range(n_seq):
            pp = [ph for ph in phases if ph[0] == p]
            if n_phases is not None:
                pp = pp[:n_phases]
            run_phases.append(pp)

        schedA = []
        for pp in run_phases:
            for (p, l, kind, f) in pp:
                if kind == "ffn":
                    for j in range(GROUPS[DBG["ng"] - 1][1]):
                        schedA.append(("gu", l, f, j))
                else:
                    order = [("pre", 0), ("pre", 1), ("out", 0), ("pre", 2), ("out", 1), ("pre", 3), ("out", 2), ("out", 3)]
                    for kind_, t in order:
                        for nm in (("u", "bg", "cg", "xin") if kind_ == "pre" else ("wo0", "wo1", "wo2", "wo3")):
                            schedA.append((nm, l, t))
        sA = {"use": 0, "load": 0}
        WINS_IDX = {"u": 0, "va": 1, "bg": 2, "cg": 3, "xin": 4}

        def ringA_src(key):
            nm = key[0]
            if nm == "gu":
                _, l, f, j = key
                return wgu_d[(l * 2 + f) * NJ + j, :, :]
            l = key[1]
            if nm.startswith("wo"):
                return wout_d[l * 4 + int(nm[2]), :, :]
            return wins_d[l * 5 + WINS_IDX[nm], :, :]

        def ringA_emit_load():
            i = sA["load"]
            if i >= len(schedA):
                return
            sA["load"] += 1
            s = i % NR
            B.dma(POOL, ringA_s[s], ringA[s], ringA_src(schedA[i]), wr=[ringA_b[s]])

        def ringA_acquire(key, ahead=0):
            i = sA["use"] + ahead
            assert ahead < NR
            assert schedA[i] == key, (schedA[i], key)
            while sA["load"] <= i:
                ringA_emit_load()
            return ringA[i % NR], ringA_b[i % NR]

        def ringA_release():
            sA["use"] += 1
            while sA["load"] < min(len(schedA), sA["use"] + NR):
                ringA_emit_load()

        B.dma(SP, cst_s, cst[:, :], cst_d[:, :], wr=[cst_b])
        B.dma(SP, cst_s, colv[:, :], colv_d[:, :], wr=[colv_b])
        B.dma(SP, cst_s, av(oD + 6144, 2048, F32), cs_d[:, :], wr=[cs_b])
        for b_ in (cst_b, colv_b, cs_b):
            b_.w = (cst_s, cst_s.cnt)
        B.op(DVE, lambda e: e.tensor_copy(out=ident_b[:, :], in_=ident_f), rd=[cst_b], wr=[idb_b])
        B.op(DVE, lambda e: e.memset(ones_b[:, :], 1.0), wr=[ones_bb])
        for _ in range(NR):
            ringA_emit_load()

        def mm(out, lhsT, rhs, start, stop, rd, wr, inc=True):
            B.op(PE, lambda e: e.matmul(out, lhsT, rhs, start=start, stop=stop), rd=rd, wr=wr, inc=inc)

        def tr(out, in_, ident, rd, wr, inc=True):
            B.op(PE, lambda e: e.transpose(out, in_, ident), rd=rd, wr=wr, inc=inc)

        def act(out, in_, func, rd, wr, bias=None, scale=None):
            kw = {}
            if bias is not None:
                kw["bias"] = bias
            if scale is not None:
                kw["scale"] = scale
            B.op(ACT, lambda e: e.activation(out=out, in_=in_, func=func, **kw), rd=rd, wr=wr)

        def tt(eng, out, in0, in1, op, rd, wr):
            B.op(eng, lambda e: e.tensor_tensor(out=out, in0=in0, in1=in1, op=op), rd=rd, wr=wr)

        def ts(eng, out, in0, s1, s2, op0, op1, rd, wr):
            if s2 is None:
                B.op(eng, lambda e: e.tensor_scalar(out=out, in0=in0, scalar1=s1, scalar2=None, op0=op0), rd=rd, wr=wr)
            else:
                B.op(eng, lambda e: e.tensor_scalar(out=out, in0=in0, scalar1=s1, scalar2=s2, op0=op0, op1=op1),
                     rd=rd, wr=wr)

        def stt(eng, out, in0, scalar, in1, op0, op1, rd, wr):
            B.op(eng, lambda e: e.scalar_tensor_tensor(out=out, in0=in0, scalar=scalar, in1=in1, op0=op0, op1=op1),
                 rd=rd, wr=wr)

        def tsl(t):
            return slice(t * TILE, (t + 1) * TILE)

        def rms_sq(t):
            bank, bb = ps_next()
            for kc in range(8):
                act(sq[kc % 4], xT[:, kc, tsl(t)], AF.Square, rd=[xb[kc][t]], wr=[sq_b[kc % 4]])
                mm(bank, ones_b[:, :], sq[kc % 4], kc == 0, kc == 7, rd=[sq_b[kc % 4], ones_bb], wr=[bb])
            return bank, bb

        def rms_sqrt(bank, bb):
            act(rt, bank, AF.Sqrt, rd=[bb], wr=[rt_b], bias=EPS, scale=1.0 / D)

        def rms_recip():
            B.op(DVE, lambda e: e.reciprocal(out=rstd, in_=rt), rd=[rt_b], wr=[rstd_b])

        def rms_stats(t):
            bank, bb = rms_sq(t)
            rms_sqrt(bank, bb)
            rms_recip()

        def rms_apply(t, gc0, dst, dst_bufs, dcols):
            for kc in range(8):
                stt(POOL if kc in POOL_KC else DVE, dst[:, kc, dcols], xT[:, kc, tsl(t)], colv[:, gc0 + kc:gc0 + kc + 1], rstd,
                    ALU.mult, ALU.mult, rd=[xb[kc][t], rstd_b, colv_b], wr=[dst_bufs[kc]])

        def rmsnorm(t, gc0, dst, dst_bufs, dcols):
            rms_stats(t)
            rms_apply(t, gc0, dst, dst_bufs, dcols)

        xio_s = [B.new_sem(f"dxio{t}") for t in range(NT)]

        def load_x(p):
            for t in range(NT):
                src = x_d[p, :, tsl(t)].rearrange("(k p) n -> p k n", p=128)
                B.dma(SP, xio_s[t], xT[:, :, tsl(t)], src, wr=[xb[k][t] for k in range(8)])

        def store_x(p, do_norm):
            for t in range(NT):
                if do_norm:
                    rms_stats(t)
                    for kc in range(8):
                        stt(DVE, xT[:, kc, tsl(t)], xT[:, kc, tsl(t)], colv[:, 60 + kc:61 + kc], rstd,
                            ALU.mult, ALU.mult, rd=[rstd_b, colv_b], wr=[xb[kc][t]])
                dst = out_d[p, :, tsl(t)].rearrange("(k p) n -> p k n", p=128)
                B.dma(SP, xio_s[t], dst, xT[:, :, tsl(t)], rd=[xb[k][t] for k in range(8)])

        def ffn(l, f):
            gc0 = l * 30 + (0 if f == 0 else 16)
            lf = l * 2 + f
            nwd = {"n": 0}

            def wd_load(gi):
                j0, j1 = GROUPS[gi]
                s = gi % 2
                src = wd_d[lf, j0 * 128:j1 * 128, :].rearrange("(j p) n -> p j n", p=128)
                B.dma(POOL, wd_s[s], wd_slots[s][:, 0:j1 - j0, :], src, wr=[wd_b[s]])

            wd_load(0)
            wd_load(1)
            rmsnorm(0, gc0, hT, [hb[k][0] for k in range(8)], tsl(0))
            sgi = 0

            def gu(j, jj, t, slot, slot_b):
                nonlocal sgi
                wv = slot.rearrange("p (k g c) -> p k g c", k=8, g=2)
                bg_, bgb = ps_next()
                for kc in range(8):
                    mm(bg_, wv[:, kc, 0, :], hT[:, kc, tsl(t)], kc == 0, kc == 7,
                       rd=[slot_b, hb[kc][t]], wr=[bgb], inc=(kc == 7))
                bu_, bub = ps_next()
                for kc in range(8):
                    mm(bu_, wv[:, kc, 1, :], hT[:, kc, tsl(t)], kc == 0, kc == 7,
                       rd=[slot_b, hb[kc][t]], wr=[bub], inc=(kc == 7))
                si = sgi % 3
                sgi += 1
                act(sg[si], bg_, AF.Silu, rd=[bgb], wr=[sg_b[si]])
                tt(DVE, aT[:, jj, tsl(t)], sg[si], bu_, ALU.mult, rd=[sg_b[si], bub], wr=[ab[jj][t]])

            for gi, (j0, j1) in enumerate(GROUPS):
                if gi >= DBG["ng"]:
                    return
                jstart = j0
                if gi == 0:
                    NI = 3
                    slots = [ringA_acquire(("gu", l, f, j0 + k), ahead=k) for k in range(NI)]
                    for t in range(NT):
                        if t < NT - 1:
                            rmsnorm(t + 1, gc0, hT, [hb[k][t + 1] for k in range(8)], tsl(t + 1))
                        for k in range(NI):
                            gu(j0 + k, k, t, *slots[k])
                    for k in range(NI):
                        ringA_release()
                    jstart = j0 + NI
                for j in range(jstart, j1):
                    jj = j - j0
                    slot, slot_b = ringA_acquire(("gu", l, f, j))
                    for t in range(NT):
                        gu(j, jj, t, slot, slot_b)
                    ringA_release()
                s = gi % 2
                if DBG["ffn"] == 2:
                    continue
                for t in range(NT):
                    for m in range(8):
                        by_, byb = ps_next()
                        n = j1 - j0
                        for jj in range(n):
                            mm(by_, wd_slots[s][:, jj, m * 128:(m + 1) * 128], aT[:, jj, tsl(t)], jj == 0, jj == n - 1,
                               rd=[wd_b[s], ab[jj][t]], wr=[byb], inc=(jj == n - 1))
                        stt(DVE, xT[:, m, tsl(t)], by_, 0.5, xT[:, m, tsl(t)], ALU.mult, ALU.add,
                            rd=[byb], wr=[xb[m][t]])
                if gi + 2 < len(GROUPS):
                    wd_load(gi + 2)

        GAMMA = [1.0 - 2.0 ** (-5.0 - h) for h in range(4)]
        GCH = [g ** CH for g in GAMMA]

        def mixer(l):
            XE = POOL if OFFLOAD else DVE
            gc0 = l * 30 + 8
            cw0 = l * 30 + 24
            X = mybir.AxisListType.X
            B.dma(SP, rowb_s, av(oD + 14336, 1024, F32), rowb_d[l, :, :], wr=[rowb_b])
            B.dma(SP, rowb_s, wsT_f, wst_d[l, :, :], wr=[wst_b])
            rowb_b.w = wst_b.w = (rowb_s, rowb_s.cnt)
            tt(DVE, wsT_b, wsT_f.rearrange("p (g n) -> p g n", g=4),
               maskT.unsqueeze(1).to_broadcast([128, 4, 128]), ALU.mult, rd=[wst_b, cst_b], wr=[wsTb_b])
            for i in range(4):
                B.dma(POOL, winc_s[i], winc_flat[i], winc_d[l * 4 + i, :, :], wr=[winc_b[i]])
            B.dma(POOL, wva_s, wva_flat, wins_d[l * 5 + 1, :, :], wr=[wva_b])
            B.op(DVE, lambda e: e.memset(vz.rearrange("p g n -> p (g n)"), 0.0), wr=[vz_b])
            for p_ in range(2):
                B.op(DVE, lambda e, o=var8[p_]: e.memset(o, 1.0), wr=[var8_b[p_]])
            st = {}

            t1q = t1.rearrange("p (h t i) -> p h t i", h=4, t=2)
            t2q = t2.rearrange("p (h t i) -> p h t i", h=4, t=2)

            def rotary(src, srcb, dst, dstb, c):
                sv_ = src.rearrange("p (h t i) -> p h t i", h=4, t=2)
                cos4 = cosT[:, c, :].unsqueeze(1).unsqueeze(1).to_broadcast([128, 4, 2, 64])
                sinb = sinT[:, c, :].unsqueeze(1).to_broadcast([128, 4, 64])
                tt(DVE, t1q, sv_, cos4, ALU.mult, rd=[srcb, cs_b], wr=[t1_b])
                stt(DVE, t2q[:, :, 0, :], sv_[:, :, 1, :], -1.0, sinb, ALU.mult, ALU.mult, rd=[srcb, cs_b], wr=[t2_b])
                tt(DVE, t2q[:, :, 1, :], sv_[:, :, 0, :], sinb, ALU.mult, rd=[srcb, cs_b], wr=[t2_b])
                tt(DVE, dst, t1, t2, ALU.add, rd=[t1_b, t2_b], wr=[dstb])

            def PRE_apply(t):
                par = t % 2
                rms_recip()
                rms_apply(t, gc0, hm2[par], [hm_b2[par]] * 8, slice(0, TILE))

            def PRE_fm(t):
                par = t % 2
                hm, hm_b = hm2[par], hm_b2[par]
                fm = {}
                for nm in ("u", "bg", "cg", "xin"):
                    slot, slot_b = ringA_acquire((nm, l, t))
                    wv = slot.rearrange("p (k c) -> p k c", k=8)
                    for ch in range(2):
                        bk, bkb = ps_next()
                        for kc in range(8):
                            mm(bk, wv[:, kc, ch * 128:(ch + 1) * 128], hm[:, kc, :], kc == 0, kc == 7,
                               rd=[slot_b, hm_b], wr=[bkb], inc=(kc == 7))
                        fm[(nm, ch)] = (bk, bkb)
                        if nm == "u":
                            act(uT[par][:, ch, :], bk, AF.Gelu_apprx_tanh, rd=[bkb], wr=[uT_b[par]])
                    ringA_release()
                for ch in range(2):
                    bgk, bgb_ = fm[("bg", ch)]
                    cgk, cgb_ = fm[("cg", ch)]
                    xik, xib_ = fm[("xin", ch)]
                    B.op(ACT, lambda e, i=cgk: e.copy(out=t1, in_=i), rd=[cgb_], wr=[t1_b])
                    if t == 0:
                        B.op(DVE, lambda e, o=zc[:, ch, 0:2]: e.memset(o, 0.0), wr=[zc_b[ch]])
                    else:
                        B.op(DVE, lambda e, o=zc[:, ch, 0:2], i=zc[:, ch, 512:514]: e.tensor_copy(out=o, in_=i),
                             rd=[zc_b[ch]], wr=[zc_b[ch]])
                    tt(DVE, zc[:, ch, 2:514], t1, xik, ALU.mult, rd=[t1_b, xib_], wr=[zc_b[ch]])
                    c0 = cw0 + ch * 3
                    ts(DVE, t2, zc[:, ch, 2:514], colv[:, c0 + 2:c0 + 3], None, ALU.mult, None,
                       rd=[zc_b[ch], colv_b], wr=[t2_b])
                    stt(DVE, t2, zc[:, ch, 1:513], colv[:, c0 + 1:c0 + 2], t2, ALU.mult, ALU.add,
                        rd=[zc_b[ch], t2_b], wr=[t2_b])
                    stt(DVE, t2, zc[:, ch, 0:512], colv[:, c0:c0 + 1], t2, ALU.mult, ALU.add,
                        rd=[zc_b[ch], t2_b], wr=[t2_b])
                    tt(DVE, ycat[par][:, 2 + ch, :], t2, bgk, ALU.mult, rd=[t2_b, bgb_], wr=[ycB_b[par]])

            def OUT(t):
                par = t % 2
                rdy = ycA_b[par] + [ycB_b[par]] + ycC_b[par]
                for wo in range(4):
                    slot, slot_b = ringA_acquire((f"wo{wo}", l, t))
                    wv = slot.rearrange("p (k c) -> p k c", k=8)
                    for mh in range(2):
                        m = wo * 2 + mh
                        bo, bob = ps_next()
                        for kc in range(8):
                            mm(bo, wv[:, kc, mh * 128:(mh + 1) * 128], ycat[par][:, kc, :], kc == 0, kc == 7,
                               rd=[slot_b] + rdy, wr=[bob], inc=(kc == 7))
                        tt(DVE, xT[:, m, tsl(t)], bo, xT[:, m, tsl(t)], ALU.add, rd=[bob], wr=[xb[m][t]])
                    ringA_release()

            def proj(c, i_or_va):
                cq = c % 4
                hm, hm_b = hm2[(c // 4) % 2], hm_b2[(c // 4) % 2]
                csl = slice(cq * 128, (cq + 1) * 128)
                bk, bkb = ps_next()
                if i_or_va == "va":
                    for kc in range(8):
                        mm(bk[:, 0:256], hm[:, kc, csl], wva[:, kc, :], kc == 0, kc == 7,
                           rd=[hm_b, wva_b], wr=[bkb], inc=(kc == 7))
                else:
                    i = i_or_va
                    for kc in range(8):
                        mm(bk, hm[:, kc, csl], winc[i][:, kc, :], kc == 0, kc == 7,
                           rd=[hm_b, winc_b[i]], wr=[bkb], inc=(kc == 7))
                return bk, bkb

            def blkB(c):
                pva, pva_b = proj(c, "va")
                pq_, pq_b = proj(c, 0)
                act(vg, pva[:, 0:256], AF.Gelu_apprx_tanh, rd=[pva_b], wr=[vg_b])
                rotary(pq_, pq_b, qrot, qrot_b, c)

            def blkD(c):
                par = c % 2
                pk_, pk_b = proj(c, 1)
                pv_, pv_b = proj(c, 2)
                B.op(ACT, lambda e, o=vtok[par], i=pv_: e.copy(out=o, in_=i), rd=[pv_b], wr=[vtok_b[par]])
                rotary(pk_, pk_b, krot, krot_b, c)
                tt(XE, kz[par].rearrange("p (h d) -> p h d", h=4), krot.rearrange("p (h d) -> p h d", h=4),
                   zs.unsqueeze(2).to_broadcast([128, 4, 128]), ALU.mult, rd=[krot_b, cst_b], wr=[kz_b[par]])

            def A_stats1(c):
                vg3 = vg.rearrange("p (g i) -> p g i", g=4)
                B.op(DVE, lambda e: e.reduce_sum(out=s1A, in_=vg3, axis=X), rd=[vg_b], wr=[s1A_b])
                stt(DVE, vg3, s1A.unsqueeze(2).to_broadcast([128, 4, 64]), -1.0 / 64, vg3, ALU.mult, ALU.add,
                    rd=[s1A_b, vg_b], wr=[vg_b])
                act(sqA, vg, AF.Square, rd=[vg_b], wr=[sqA_b], scale=0.125)

            def A_stats2(c, p8):
                B.op(DVE, lambda e, o=var8[p8][:, 0:4]: e.reduce_sum(out=o, in_=sqA.rearrange("p (g i) -> p g i", g=4), axis=X),
                     rd=[sqA_b], wr=[var8_b[p8]])

            def A_finish(c, p8):
                vg3 = vg.rearrange("p (g i) -> p g i", g=4)
                tt(XE, vg3, vg3, rs8[p8][:, 0:4].unsqueeze(2).to_broadcast([128, 4, 64]), ALU.mult,
                   rd=[vg_b, rs8_b[p8]], wr=[vg_b])
                vzp = vz.rearrange("p (a e) n -> p a e n", e=2)
                vgp = vg.rearrange("p (a e i) -> p a e i", a=2, e=2)
                vnp = vnorm_b.rearrange("p (a e i) -> p a e i", a=2, e=2)
                for e_ in range(2):
                    tt(XE, vzp[:, :, e_, e_ * 64:(e_ + 1) * 64], vgp[:, :, e_, :], vnp[:, :, e_, :], ALU.mult,
                       rd=[vg_b, rowb_b], wr=[vz_b])

            def blkF(c):
                par = c % 2
                pg_, pg_b = proj(c, 3)
                act(sgc[par], pg_, AF.Tanh, rd=[pg_b], wr=[sgc_b[par]], scale=0.5)
                st[("pg", c)] = (pg_, pg_b)

            def blkF2(c):
                par = c % 2
                pg_, pg_b = st[("pg", c)]
                stt(DVE, sgc[par], sgc[par], 1.0, pg_, ALU.add, ALU.mult, rd=[sgc_b[par], pg_b], wr=[sgc_b[par]])

            def blkA(c):
                bk, bkb = ps_next()
                bkv = bk.bitcast(BF16)
                for h in range(4):
                    tr(bkv[:, h * 128:(h + 1) * 128], qrot[:, h * 128:(h + 1) * 128], ident_b[:, :],
                       rd=[qrot_b, idb_b], wr=[bkb], inc=False)
                for h in range(4):
                    tr(bkv[:, 512 + h * 128:512 + (h + 1) * 128], krot[:, h * 128:(h + 1) * 128], ident_b[:, :],
                       rd=[krot_b, idb_b], wr=[bkb], inc=(h == 3))
                B.op(ACT, lambda e, i=bkv: e.copy(out=qkT, in_=i), rd=[bkb], wr=[qkT_b])

            def gmlp(c):
                t = c // 4
                cq = c % 4
                part = t % 2
                csl = slice(cq * 128, (cq + 1) * 128)
                psv, psv_b = ps_next()
                for g in range(4):
                    mm(psv[:, (g // 2) * 128:(g // 2 + 1) * 128], vz[:, g, :], wsT_b[:, g, :],
                       g % 2 == 0, g % 2 == 1, rd=[vz_b, wsTb_b], wr=[psv_b], inc=(g == 3))
                tt(DVE, svt.rearrange("p (k n) -> p k n", k=2), psv[:, 0:256].rearrange("p (k n) -> p k n", k=2),
                   bsb, ALU.add, rd=[psv_b, rowb_b], wr=[svt_b])
                tt(DVE, ycat[part][:, 0:2, csl], svt.rearrange("p (k n) -> p k n", k=2), uT[part][:, :, csl], ALU.mult,
                   rd=[svt_b, uT_b[part]], wr=[ycA_b[part][cq]])

            def blkC(c):
                pss, pss_b = ps_next()
                for h in range(4):
                    mm(pss[:, h * 128:(h + 1) * 128], qkT[:, 512 + h * 128:512 + (h + 1) * 128],
                       qkT[:, h * 128:(h + 1) * 128], True, True, rd=[qkT_b], wr=[pss_b], inc=(h == 3))
                tt(DVE, sT, pss.rearrange("p (h n) -> p h n", h=4), M2, ALU.mult, rd=[pss_b, cst_b], wr=[sT_b])

            def blkE(c):
                par = c % 2
                pr, pr_b = ps_next()
                for h in range(4):
                    o = pr[:, h * 128:(h + 1) * 128]
                    if c == 0:
                        mm(o, sT[:, h, :], vtok[par][:, h * 128:(h + 1) * 128], True, True,
                           rd=[sT_b, vtok_b[par]], wr=[pr_b], inc=(h == 3))
                    else:
                        mm(o, sT[:, h, :], vtok[par][:, h * 128:(h + 1) * 128], True, False,
                           rd=[sT_b, vtok_b[par]], wr=[pr_b], inc=False)
                        mm(o, qkT[:, h * 128:(h + 1) * 128], Sbf[:, h, :], False, True,
                           rd=[qkT_b, Sbf_b], wr=[pr_b], inc=(h == 3))
                st[c] = (pr, pr_b)
                if c < NCH - 1:
                    pkv, pkv_b = ps_next()
                    for h in range(4):
                        mm(pkv[:, h * 128:(h + 1) * 128], kz[par][:, h * 128:(h + 1) * 128],
                           vtok[par][:, h * 128:(h + 1) * 128], True, True,
                           rd=[kz_b[par], vtok_b[par]], wr=[pkv_b], inc=(h == 3))
                    pkv3 = pkv.rearrange("p (h n) -> p h n", h=4)
                    if c == 0:
                        B.op(DVE, lambda e, i=pkv3: e.tensor_copy(out=Sst, in_=i), rd=[pkv_b], wr=[S_b])
                    else:
                        tt(DVE, Sst, Sst, pkv3, ALU.add, rd=[S_b, pkv_b], wr=[S_b])
                    B.op(ACT, lambda e: e.copy(out=Sbf, in_=Sst), rd=[S_b], wr=[Sbf_b])
                    if c < NCH - 2:
                        tt(XE, Sst, Sst, gch_c.unsqueeze(2).to_broadcast([128, 4, 128]), ALU.mult,
                           rd=[S_b, cst_b], wr=[S_b])

            def C_stats1(c):
                pr, pr_b = st[c]
                pr3 = pr.rearrange("p (h n) -> p h n", h=4)
                rn3 = rn.rearrange("p (h n) -> p h n", h=4)
                B.op(DVE, lambda e, i=pr3: e.reduce_sum(out=s1C, in_=i, axis=X), rd=[pr_b], wr=[s1C_b])
                stt(DVE, rn3, s1C.unsqueeze(2).to_broadcast([128, 4, 128]), -1.0 / 128, pr3, ALU.mult, ALU.add,
                    rd=[s1C_b, pr_b], wr=[rn_b])
                act(sqC, rn, AF.Square, rd=[rn_b], wr=[sqC_b], scale=128.0 ** -0.5)

            def C_stats2(c, p8):
                B.op(DVE, lambda e, o=var8[p8][:, 4:8]: e.reduce_sum(out=o, in_=sqC.rearrange("p (h n) -> p h n", h=4), axis=X),
                     rd=[sqC_b], wr=[var8_b[p8]])

            def joint_a(p8):
                tt(DVE, var8[p8], var8[p8], eps8, ALU.add, rd=[var8_b[p8], cst_b], wr=[var8_b[p8]])
                act(rs8[p8], var8[p8], AF.Sqrt, rd=[var8_b[p8]], wr=[rs8_b[p8]])

            def joint_b(p8):
                B.op(DVE, lambda e, o=rs8[p8]: e.reciprocal(out=o, in_=o), rd=[rs8_b[p8]], wr=[rs8_b[p8]])
                ts(DVE, rs8[p8][:, 4:8], rs8[p8][:, 4:8], 0.5, None, ALU.mult, None, rd=[rs8_b[p8]], wr=[rs8_b[p8]])

            def C_finish(c, p8):
                par = c % 2
                rn3 = rn.rearrange("p (h n) -> p h n", h=4)
                tt(XE, rn3, rn3, rs8[p8][:, 4:8].unsqueeze(2).to_broadcast([128, 4, 128]), ALU.mult,
                   rd=[rn_b, rs8_b[p8]], wr=[rn_b])
                tt(XE, rn, rn, gn_b, ALU.mult, rd=[rn_b, rowb_b], wr=[rn_b])
                tt(XE, yc[par], rn, sgc[par], ALU.mult, rd=[rn_b, sgc_b[par]], wr=[yc_b[par]])

            def blkG(c):
                t = c // 4
                cq = c % 4
                part = t % 2
                par = c % 2
                csl = slice(cq * 128, (cq + 1) * 128)
                bk, bkb = ps_next()
                bkv = bk.bitcast(BF16)
                for h in range(4):
                    tr(bkv[:, h * 128:(h + 1) * 128], yc[par][:, h * 128:(h + 1) * 128], ident_b[:, :],
                       rd=[yc_b[par], idb_b], wr=[bkb], inc=(h == 3))
                B.op(ACT, lambda e, o=ycat[part][:, 4:8, csl], i=bkv[:, 0:512].rearrange("p (h n) -> p h n", h=4):
                     e.copy(out=o, in_=i), rd=[bkb], wr=[ycC_b[part][cq]])

            bank0, bb0 = rms_sq(0)
            rms_sqrt(bank0, bb0)
            PRE_apply(0)
            PRE_fm(0)
            for i in range(NCH + 2):
                p8 = i % 2
                cur = i < NCH
                prv = 0 <= i - 1 < NCH
                nrm = cur and i % 4 == 1 and i + 3 < NCH
                if cur:
                    blkB(i)
                if prv:
                    blkC(i - 1)
                if cur:
                    blkD(i)
                if nrm:
                    nbank, nbb = rms_sq((i + 3) // 4)
                if cur:
                    A_stats1(i)
                if prv:
                    blkE(i - 1)
                    C_stats1(i - 1)
                if cur:
                    A_stats2(i, p8)
                if prv:
                    gmlp(i - 1)
                if cur:
                    blkF(i)
                    blkA(i)
                if prv:
                    C_stats2(i - 1, p8)
                if cur or prv:
                    joint_a(p8)
                if nrm:
                    rms_sqrt(nbank, nbb)
                if cur:
                    blkF2(i)
                if cur or prv:
                    joint_b(p8)
                if cur:
                    A_finish(i, p8)
                if prv:
                    C_finish(i - 1, p8)
                if nrm:
                    PRE_apply((i + 3) // 4)
                if 0 <= i - 2 < NCH:
                    blkG(i - 2)
                    if (i - 2) % 4 == 3:
                        OUT((i - 2) // 4)
                if cur and i % 4 == 2 and i + 2 < NCH:
                    PRE_fm((i + 2) // 4)

        cur_B, cur_C, cur_D = [], [], []

        def set_regions(newB, newC, newD=None):
            nonlocal cur_B, cur_C, cur_D
            if newB is not cur_B:
                alias_switch(cur_B, newB)
                cur_B = newB
            if newC is not cur_C:
                alias_switch(cur_C, newC)
                cur_C = newC
            if newD is not None and newD is not cur_D:
                alias_switch(cur_D, newD)
                cur_D = newD

        for p in range(n_seq):
            load_x(p)
            for (_, l, kind, f) in run_phases[p]:
                if kind == "ffn":
                    set_regions(regB_ffn, regC_ffn, regD_ffn)
                    ffn(l, f)
                else:
                    set_regions(regB_mix, regC_mix, regD_mix)
                    mixer(l)
            store_x(p, final_norm and n_phases is None)
        B.op(SP, lambda e: e.nop(), wr=[xb[k][t] for k in range(8) for t in range(NT)])

        with nc.Block() as block:
            @block.tensor
            def _(e):
                _replay(e, PE.ops)

            @block.scalar
            def _(e):
                _replay(e, ACT.ops)

            @block.vector
            def _(e):
                _replay(e, DVE.ops)

            @block.gpsimd
            def _(e):
                _replay(e, POOL.ops)

            @block.sync
            def _(e):
                _replay(e, SP.ops)
        stats = {k.name: len(k.ops) for k in (PE, ACT, DVE, POOL, SP)}
        nc._k_stats = stats
    return nc


def _consts():
    idx = np.arange(CH, dtype=np.float64)
    gam = np.array([1.0 - 2.0 ** (-5.0 - h) for h in range(4)], dtype=np.float64)
    logg = np.log(gam)
    scale = 128.0 ** -0.5
    cst = np.zeros((128, NCST), np.float64)
    cst[:, 0:128] = np.eye(128)
    s = idx[:, None]
    t = idx[None, :]
    for h in range(4):
        cst[:, 128 + h * 128:128 + (h + 1) * 128] = (s <= t) * np.exp(-logg[h] * (s + 1.0)) * scale
    cst[:, 640:768] = (s <= t)
    for h in range(4):
        cst[:, 768 + h] = np.exp(logg[h] * (CH - 1 - idx)) * scale
        cst[:, 772 + h] = EPS / np.exp(logg[h] * (idx + 1.0)) ** 2
        cst[:, 776 + h] = gam[h] ** CH
        cst[:, 780 + h] = EPS
        cst[:, 784 + h] = EPS / np.exp(logg[h] * (idx + 1.0)) ** 2
    half = 64
    inv = (10000.0 ** (-np.arange(half, dtype=np.float32) / half)).astype(np.float32)
    pos = np.arange(SEQ, dtype=np.float32)
    ang = (pos[:, None] * inv[None, :]).astype(np.float32)
    cos = np.cos(ang.astype(np.float64)).reshape(NCH, 128, half).transpose(1, 0, 2).reshape(128, NCH * half)
    sin = np.sin(ang.astype(np.float64)).reshape(NCH, 128, half).transpose(1, 0, 2).reshape(128, NCH * half)
    cs = np.concatenate([cos, sin], axis=1)
    return cst.astype(np.float32), cs.astype(np.float32)


def _prep_weights(inp):
    f = lambda a: np.ascontiguousarray(np.asarray(a, dtype=np.float32))
    wg = [f(inp["ffn1_w_gate"]), f(inp["ffn2_w_gate"])]
    wu = [f(inp["ffn1_w_up"]), f(inp["ffn2_w_up"])]
    wdn = [f(inp["ffn1_w_down"]), f(inp["ffn2_w_down"])]
    wgu = np.empty((L, 2, NJ, 128, 8, 2, 128), np.float32)
    wd = np.empty((L, 2, DFF, D), np.float32)
    for l in range(L):
        for ff in range(2):
            g = wg[ff][l].reshape(8, 128, NJ, 128).transpose(2, 1, 0, 3)
            u = wu[ff][l].reshape(8, 128, NJ, 128).transpose(2, 1, 0, 3)
            wgu[l, ff, :, :, :, 0, :] = g
            wgu[l, ff, :, :, :, 1, :] = u
            wd[l, ff] = wdn[ff][l]
    w_in = f(inp["w_in"])
    wins = np.empty((L, 5, 128, 8, 256), np.float32)
    winc = np.empty((L, 4, 128, 8, 512), np.float32)
    for l in range(L):
        for i in range(5):
            wins[l, i] = w_in[l][:, i * 256:(i + 1) * 256].reshape(8, 128, 256).transpose(1, 0, 2)
        for i in range(4):
            winc[l, i] = w_in[l][:, 1280 + i * 512:1280 + (i + 1) * 512].reshape(8, 128, 512).transpose(1, 0, 2)
    w_out = f(inp["w_out"])
    wout = np.empty((L, 4, 128, 8, 256), np.float32)
    for l in range(L):
        for i in range(4):
            wout[l, i] = w_out[l][:, i * 256:(i + 1) * 256].reshape(8, 128, 256).transpose(1, 0, 2)
    colv = np.zeros((128, NCV), np.float32)
    for l in range(L):
        colv[:, l * 30 + 0:l * 30 + 8] = f(inp["ffn1_norm"])[l].reshape(8, 128).T
        colv[:, l * 30 + 8:l * 30 + 16] = f(inp["mix_norm"])[l].reshape(8, 128).T
        colv[:, l * 30 + 16:l * 30 + 24] = f(inp["ffn2_norm"])[l].reshape(8, 128).T
        cw = f(inp["conv_w"])[l]
        for ch in range(2):
            for i in range(3):
                colv[:, l * 30 + 24 + ch * 3 + i] = cw[i, ch * 128:(ch + 1) * 128]
    colv[:, 60:68] = f(inp["final_norm"]).reshape(8, 128).T
    rowb = np.empty((L, 128, 1024), np.float32)
    bs = f(inp["gmlp_b_s"])
    for l in range(L):
        rowb[l, :, 0:256] = f(inp["gmlp_v_norm"])[l][None, :]
        rowb[l, :, 256:768] = f(inp["ret_gn"])[l][None, :]
        for ch in range(2):
            rowb[l, 0:64, 768 + ch * 128:768 + (ch + 1) * 128] = bs[l, 2 * ch][None, :]
            rowb[l, 64:128, 768 + ch * 128:768 + (ch + 1) * 128] = bs[l, 2 * ch + 1][None, :]
    wst = np.ascontiguousarray(f(inp["gmlp_w_s"]).transpose(0, 3, 1, 2)).reshape(L, 128, 512)
    cst, cs = _consts()
    return {
        "wgu": wgu.reshape(L * 2 * NJ, 128, 2048),
        "wd": wd.reshape(L * 2, DFF, D),
        "wins": wins.reshape(L * 5, 128, 2048),
        "winc": winc.reshape(L * 4, 128, 4096),
        "wout": wout.reshape(L * 4, 128, 2048),
        "colv": colv, "rowb": rowb, "wst": wst, "cst": cst, "cossin": cs,
    }


_NC_CACHE = {}


def kernel(**inputs):
    return _run(inputs)


def _run(inputs, n_phases=None, final_norm=True, n_cores=8):
    x = np.ascontiguousarray(np.asarray(inputs["x"], dtype=np.float32))
    shared = _prep_weights(inputs)
    key = (n_phases, final_norm)
    if key not in _NC_CACHE:
        _NC_CACHE[key] = build(n_phases, final_norm)
    nc = _NC_CACHE[key]
    n = n_cores
    in_maps = []
    for c in range(n):
        m = dict(shared)
        m["x"] = np.ascontiguousarray(x[c * NSEQ:(c + 1) * NSEQ].transpose(0, 2, 1))
        in_maps.append(m)
    res = run_bass_kernel_spmd(nc, in_maps, core_ids=list(range(n)))
    out = np.concatenate([np.asarray(r["out"]).transpose(0, 2, 1) for r in res.results], axis=0)
    out = np.ascontiguousarray(out)
    return out.astype(np.float32, copy=False)
```

```python
import math
from contextlib import ExitStack

import numpy as np
import concourse.bass as bass
import concourse.mybir as mybir
from concourse.bass_utils import run_bass_kernel_spmd

F32 = mybir.dt.float32
BF16 = mybir.dt.bfloat16
AF = mybir.ActivationFunctionType
ALU = mybir.AluOpType

D = 1024
SEQ = 2048
NSEQ = 2
L = 2
DFF = 2816
NJ = 22
TILE = 512
NT = SEQ // TILE
CH = 128
NCH = SEQ // CH
GROUPS = [(0, 6), (6, 12), (12, 17), (17, 22)]
EPS = 1e-6
NCV = 68
NCST = 128 + 512 + 128 + 4 + 4 + 4 + 8
SEM_LIMIT = 3000
DBG = {"ffn": 9, "ng": 4}
POOL_KC = ()
OFFLOAD = True


class SemH:
    __slots__ = ("h", "eng", "cnt")

    def __init__(self, h, eng=None):
        self.h = h
        self.eng = eng
        self.cnt = 0


class Buf:
    __slots__ = ("w", "rd", "name")

    def __init__(self, name=""):
        self.w = None
        self.rd = {}
        self.name = name


class Eng:
    def __init__(self, name, sems, is_pe=False):
        self.name = name
        self.sems = sems
        for s in sems:
            s.eng = self
        self.si = 0
        self.ops = []
        self.known = {}
        self.is_pe = is_pe

    def cur(self):
        return self.sems[self.si]


class Builder:
    def __init__(self, nc, es):
        self.nc = nc
        self.es = es
        self.sem_i = 0

    def new_sem(self, name=None):
        self.sem_i += 1
        return SemH(self.es.enter_context(self.nc.semaphore(name or f"s{self.sem_i}")))

    def mk_eng(self, name, nsem, is_pe=False):
        return Eng(name, [self.new_sem(f"{name}{i}") for i in range(nsem)], is_pe)

    def _collect(self, eng, rd, wr):
        waits = {}

        def need(s, v):
            if s.eng is eng and eng.is_pe:
                return
            if eng.known.get(s, 0) >= v:
                return
            if waits.get(s, 0) < v:
                waits[s] = v

        for b in rd:
            if b.w is not None:
                need(*b.w)
        for b in wr:
            if b.w is not None:
                need(*b.w)
            for s, v in b.rd.items():
                need(s, v)
        for s, v in waits.items():
            eng.known[s] = v
        return [(s.h, v) for s, v in waits.items()]

    @staticmethod
    def _record(tag, rd, wr):
        s, v = tag
        for b in rd:
            if b.rd.get(s, 0) < v:
                b.rd[s] = v
        for b in wr:
            b.w = tag
            b.rd = {}

    def op(self, eng, fn, rd=(), wr=(), inc=True):
        waits = self._collect(eng, rd, wr)
        sem = eng.cur()
        if inc:
            sem.cnt += 1
            tag = (sem, sem.cnt)
            eng.ops.append((waits, fn, sem.h, 1))
            if sem.cnt >= SEM_LIMIT:
                eng.si += 1
        else:
            tag = (sem, sem.cnt + 1)
            eng.ops.append((waits, fn, None, 0))
        self._record(tag, rd, wr)

    def dma(self, q, dsem, out, in_, rd=(), wr=()):
        waits = self._collect(q, rd, wr)
        dsem.cnt += 16
        tag = (dsem, dsem.cnt)
        q.ops.append((waits, lambda e: e.dma_start(out=out, in_=in_), dsem.h, 16))
        self._record(tag, rd, wr)


def _replay(e, ops):
    for waits, fn, inc, amt in ops:
        for h, v in waits:
            e.wait_ge(h, v)
        ins = fn(e)
        if inc is not None:
            ins.then_inc(inc, amt)


def alias_switch(old, new):
    tags = {}
    for b in old:
        if b.w is not None:
            s, v = b.w
            if tags.get(s, 0) < v:
                tags[s] = v
        for s, v in b.rd.items():
            if tags.get(s, 0) < v:
                tags[s] = v
    for b in new:
        b.w = None
        b.rd = dict(tags)


def build(n_phases=None, final_norm=True, n_seq=NSEQ):
    nc = bass.Bass("TRN2", target_bir_lowering=False)

    def dram(name, shape, kind="ExternalInput"):
        return nc.dram_tensor(name, shape, F32, kind=kind).ap()

    x_d = dram("x", [NSEQ, D, SEQ])
    wgu_d = dram("wgu", [L * 2 * NJ, 128, 2048])
    wd_d = dram("wd", [L * 2, DFF, D])
    wins_d = dram("wins", [L * 5, 128, 2048])
    winc_d = dram("winc", [L * 4, 128, 4096])
    wout_d = dram("wout", [L * 4, 128, 2048])
    colv_d = dram("colv", [128, NCV])
    rowb_d = dram("rowb", [L, 128, 1024])
    wst_d = dram("wst", [L, 128, 512])
    cst_d = dram("cst", [128, NCST])
    cs_d = dram("cossin", [128, 2048])
    out_d = dram("out", [NSEQ, D, SEQ], kind="ExternalOutput")

    es = ExitStack()
    with es:
        B = Builder(nc, es)
        PE = B.mk_eng("pe", 6, is_pe=True)
        ACT = B.mk_eng("act", 6)
        DVE = B.mk_eng("dve", 8)
        POOL = B.mk_eng("pool", 2)
        SP = B.mk_eng("sp", 2)

        def sb(name, shape, dt):
            return es.enter_context(nc.sbuf_tensor(name, shape, dt))

        xT_t = sb("xT", [128, 8 * SEQ], F32)
        xT = xT_t[:].rearrange("p (k n) -> p k n", k=8)
        cst = sb("cst_sb", [128, NCST], F32)
        ident_f = cst[:, 0:128]
        M2 = cst[:, 128:640].rearrange("p (h t) -> p h t", h=4)
        maskT = cst[:, 640:768]
        zs = cst[:, 768:772]
        epsxi = cst[:, 772:776]
        gch_c = cst[:, 776:780]
        eps8 = cst[:, 780:788]
        colv = sb("colv_sb", [128, NCV], F32)
        ident_b = sb("identb", [128, 128], BF16)
        ones_b = sb("onesb", [128, 128], BF16)

        NR = 3
        SZ_B = 41024
        SZ_C = 63488
        SZ_D = 24576
        ARENA = NR * 4096 + SZ_B + SZ_C + SZ_D
        arena = sb("arena", [128, ARENA // 2], BF16)

        def av(off, n, dt):
            if dt is F32:
                return arena[:, off // 2:(off + 4 * n) // 2].bitcast(F32)
            return arena[:, off // 2:(off + 2 * n) // 2]

        ringA = [av(i * 4096, 2048, BF16) for i in range(NR)]
        oB = NR * 4096
        wd_slots = [av(oB + i * 12288, 6144, BF16).rearrange("p (j n) -> p j n", j=6) for i in range(2)]
        hm2 = [av(oB + i * 8192, 4096, BF16).rearrange("p (k n) -> p k n", k=8) for i in range(2)]
        ycat = [av(oB + 16384 + i * 8192, 4096, BF16).rearrange("p (k n) -> p k n", k=8) for i in range(2)]
        uT = [av(oB + 32768 + i * 2048, 1024, BF16).rearrange("p (k n) -> p k n", k=2) for i in range(2)]
        zc = av(oB + 36864, 1032, F32).rearrange("p (k n) -> p k n", k=2)
        assert 36864 + 4128 <= SZ_B
        oC = oB + SZ_B
        hT = av(oC, 8 * SEQ, BF16).rearrange("p (k n) -> p k n", k=8)
        aT = av(oC + 32768, 6 * SEQ, BF16).rearrange("p (k n) -> p k n", k=6)
        stg = [av(oC + i * 4096, 1024, F32) for i in range(2)]
        winc_flat = [av(oC + i * 8192, 4096, BF16) for i in range(4)]
        winc = [w_.rearrange("p (k n) -> p k n", k=8) for w_ in winc_flat]
        wva_flat = av(oC + 32768, 2048, BF16)
        wva = wva_flat.rearrange("p (k n) -> p k n", k=8)
        oK = oC + 32768 + 4096
        _ko = [oK]

        def kalloc(n, dt):
            o = _ko[0]
            nb = n * (4 if dt is F32 else 2)
            _ko[0] = o + ((nb + 31) // 32) * 32
            return av(o, n, dt)

        vg = kalloc(256, F32)
        sqA = kalloc(256, BF16)
        vz = kalloc(512, BF16).rearrange("p (g n) -> p g n", g=4)
        vtok = [kalloc(512, BF16) for _ in range(2)]
        sgc = [kalloc(512, F32) for _ in range(2)]
        t1 = kalloc(512, F32)
        t2 = kalloc(512, F32)
        qrot = kalloc(512, BF16)
        krot = kalloc(512, BF16)
        kz = [kalloc(512, BF16) for _ in range(2)]
        qkT = kalloc(1024, BF16)
        sT = kalloc(512, BF16).rearrange("p (h n) -> p h n", h=4)
        rn = kalloc(512, F32)
        sqC = kalloc(512, BF16)
        yc = [kalloc(512, BF16) for _ in range(2)]
        svt = kalloc(256, F32)
        s1A = kalloc(4, F32)
        s1C = kalloc(4, F32)
        var8 = [kalloc(8, F32) for _ in range(2)]
        rs8 = [kalloc(8, F32) for _ in range(2)]
        assert _ko[0] <= oC + SZ_C, _ko[0] - oC
        oD = oC + SZ_C
        sq = [av(oD + i * 1024, 512, BF16) for i in range(4)]
        rstd = av(oD + 4096, 512, F32)
        rt = rstd
        cosT = av(oD + 6144, 1024, F32).rearrange("p (c i) -> p c i", c=16)
        sinT = av(oD + 10240, 1024, F32).rearrange("p (c i) -> p c i", c=16)
        vnorm_b = av(oD + 14336, 256, F32)
        gn_b = av(oD + 15360, 512, F32)
        bsb = av(oD + 17408, 256, F32).rearrange("p (k n) -> p k n", k=2)
        oD2 = oD + 18432
        sg = [av(oD2 + i * 2048, 512, F32) for i in range(3)]
        Sst = av(oD2, 512, F32).rearrange("p (h n) -> p h n", h=4)
        Sbf = av(oD2 + 2048, 512, BF16).rearrange("p (h n) -> p h n", h=4)
        wsT_b = av(oD2 + 3072, 512, BF16).rearrange("p (g n) -> p g n", g=4)
        wsT_f = av(oD2 + 4096, 512, F32)
        assert oD + SZ_D == ARENA

        banks = [es.enter_context(nc.psum_tensor(f"ps{i}", [128, 512], F32)) for i in range(8)]
        bank_bufs = [Buf(f"ps{i}") for i in range(8)]
        _bi = [0]

        def ps_next():
            i = _bi[0] % 8
            _bi[0] += 1
            return banks[i][:], bank_bufs[i]

        xb = [[Buf(f"x{k}_{t}") for t in range(NT)] for k in range(8)]
        hb = [[Buf() for t in range(NT)] for k in range(8)]
        ab = [[Buf() for t in range(NT)] for k in range(6)]
        ringA_b = [Buf(f"ringA{i}") for i in range(NR)]
        ringA_s = [B.new_sem(f"dA{i}") for i in range(NR)]
        wd_b = [Buf(), Buf()]
        wd_s = [B.new_sem("dwd0"), B.new_sem("dwd1")]
        winc_b = [Buf() for _ in range(4)]
        winc_s = [B.new_sem(f"dwc{i}") for i in range(4)]
        wva_b = Buf()
        wva_s = B.new_sem("dwva")
        stg_b = [Buf(), Buf()]
        stg_s = [B.new_sem("dst0"), B.new_sem("dst1")]
        sq_b = [Buf() for _ in range(4)]
        rstd_b = Buf()
        rt_b = rstd_b
        sg_b = [Buf() for _ in range(3)]
        cst_b, colv_b, cs_b, rowb_b, wst_b = Buf(), Buf(), Buf(), Buf(), Buf()
        cst_s, rowb_s = B.new_sem("dcst"), B.new_sem("drowb")
        idb_b, ones_bb = Buf(), Buf()
        hm_b2 = [Buf(), Buf()]
        ycA_b = [[Buf() for _ in range(4)] for _ in range(2)]
        ycB_b = [Buf(), Buf()]
        ycC_b = [[Buf() for _ in range(4)] for _ in range(2)]
        uT_b = [Buf(), Buf()]
        zc_b = [Buf(), Buf()]
        vg_b, sqA_b, vz_b, t1_b, t2_b, rn_b, sqC_b, svt_b = (Buf() for _ in range(8))
        vtok_b = [Buf(), Buf()]
        sgc_b = [Buf(), Buf()]
        qrot_b, krot_b, qkT_b, sT_b = Buf(), Buf(), Buf(), Buf()
        kz_b = [Buf(), Buf()]
        yc_b = [Buf(), Buf()]
        s1A_b, s1C_b = Buf(), Buf()
        var8_b = [Buf(), Buf()]
        rs8_b = [Buf(), Buf()]
        S_b, Sbf_b, wsTb_b = Buf(), Buf(), Buf()

        regB_ffn = wd_b
        regB_mix = hm_b2 + ycB_b + uT_b + zc_b + [b for r in ycA_b for b in r] + [b for r in ycC_b for b in r]
        regC_ffn = [b for row in hb for b in row] + [b for row in ab for b in row]
        regC_mix = winc_b + [wva_b, vg_b, sqA_b, vz_b, t1_b, t2_b, rn_b, sqC_b, svt_b, qrot_b, krot_b, qkT_b, sT_b,
                             s1A_b, s1C_b] + vtok_b + sgc_b + kz_b + yc_b + var8_b + rs8_b
        regC_io = stg_b
        regD_ffn = sg_b
        regD_mix = [S_b, Sbf_b, wst_b, wsTb_b]

        phases = []
        for p in range(n_seq):
            for l in range(L):
                phases += [(p, l, "ffn", 0), (p, l, "mix", 0), (p, l, "ffn", 1)]
        run_phases = []
        for p in range(n_seq):
            pp = [ph for ph in phases if ph[0] == p]
            if n_phases is not None:
                pp = pp[:n_phases]
            run_phases.append(pp)

        schedA = []
        for pp in run_phases:
            for (p, l, kind, f) in pp:
                if kind == "ffn":
                    for j in range(GROUPS[DBG["ng"] - 1][1]):
                        schedA.append(("gu", l, f, j))
                else:
                    order = [("pre", 0), ("pre", 1), ("out", 0), ("pre", 2), ("out", 1), ("pre", 3), ("out", 2), ("out", 3)]
                    for kind_, t in order:
                        for nm in (("u", "bg", "cg", "xin") if kind_ == "pre" else ("wo0", "wo1", "wo2", "wo3")):
                            schedA.append((nm, l, t))
        sA = {"use": 0, "load": 0}
        WINS_IDX = {"u": 0, "va": 1, "bg": 2, "cg": 3, "xin": 4}

        def ringA_src(key):
            nm = key[0]
            if nm == "gu":
                _, l, f, j = key
                return wgu_d[(l * 2 + f) * NJ + j, :, :]
            l = key[1]
            if nm.startswith("wo"):
                return wout_d[l * 4 + int(nm[2]), :, :]
            return wins_d[l * 5 + WINS_IDX[nm], :, :]

        def ringA_emit_load():
            i = sA["load"]
            if i >= len(schedA):
                return
            sA["load"] += 1
            s = i % NR
            B.dma(POOL, ringA_s[s], ringA[s], ringA_src(schedA[i]), wr=[ringA_b[s]])

        def ringA_acquire(key, ahead=0):
            i = sA["use"] + ahead
            assert ahead < NR
            assert schedA[i] == key, (schedA[i], key)
            while sA["load"] <= i:
                ringA_emit_load()
            return ringA[i % NR], ringA_b[i % NR]

        def ringA_release():
            sA["use"] += 1
            while sA["load"] < min(len(schedA), sA["use"] + NR):
                ringA_emit_load()

        B.dma(SP, cst_s, cst[:, :], cst_d[:, :], wr=[cst_b])
        B.dma(SP, cst_s, colv[:, :], colv_d[:, :], wr=[colv_b])
        B.dma(SP, cst_s, av(oD + 6144, 2048, F32), cs_d[:, :], wr=[cs_b])
        for b_ in (cst_b, colv_b, cs_b):
            b_.w = (cst_s, cst_s.cnt)
        B.op(DVE, lambda e: e.tensor_copy(out=ident_b[:, :], in_=ident_f), rd=[cst_b], wr=[idb_b])
        B.op(DVE, lambda e: e.memset(ones_b[:, :], 1.0), wr=[ones_bb])
        for _ in range(NR):
            ringA_emit_load()

        def mm(out, lhsT, rhs, start, stop, rd, wr, inc=True):
            B.op(PE, lambda e: e.matmul(out, lhsT, rhs, start=start, stop=stop), rd=rd, wr=wr, inc=inc)

        def tr(out, in_, ident, rd, wr, inc=True):
            B.op(PE, lambda e: e.transpose(out, in_, ident), rd=rd, wr=wr, inc=inc)

        def act(out, in_, func, rd, wr, bias=None, scale=None):
            kw = {}
            if bias is not None:
                kw["bias"] = bias
            if scale is not None:
                kw["scale"] = scale
            B.op(ACT, lambda e: e.activation(out=out, in_=in_, func=func, **kw), rd=rd, wr=wr)

        def tt(eng, out, in0, in1, op, rd, wr):
            B.op(eng, lambda e: e.tensor_tensor(out=out, in0=in0, in1=in1, op=op), rd=rd, wr=wr)

        def ts(eng, out, in0, s1, s2, op0, op1, rd, wr):
            if s2 is None:
                B.op(eng, lambda e: e.tensor_scalar(out=out, in0=in0, scalar1=s1, scalar2=None, op0=op0), rd=rd, wr=wr)
            else:
                B.op(eng, lambda e: e.tensor_scalar(out=out, in0=in0, scalar1=s1, scalar2=s2, op0=op0, op1=op1),
                     rd=rd, wr=wr)

        def stt(eng, out, in0, scalar, in1, op0, op1, rd, wr):
            B.op(eng, lambda e: e.scalar_tensor_tensor(out=out, in0=in0, scalar=scalar, in1=in1, op0=op0, op1=op1),
                 rd=rd, wr=wr)

        def tsl(t):
            return slice(t * TILE, (t + 1) * TILE)

        def rms_sq(t):
            bank, bb = ps_next()
            for kc in range(8):
                act(sq[kc % 4], xT[:, kc, tsl(t)], AF.Square, rd=[xb[kc][t]], wr=[sq_b[kc % 4]])
                mm(bank, ones_b[:, :], sq[kc % 4], kc == 0, kc == 7, rd=[sq_b[kc % 4], ones_bb], wr=[bb])
            return bank, bb

        def rms_sqrt(bank, bb):
            act(rt, bank, AF.Sqrt, rd=[bb], wr=[rt_b], bias=EPS, scale=1.0 / D)

        def rms_recip():
            B.op(DVE, lambda e: e.reciprocal(out=rstd, in_=rt), rd=[rt_b], wr=[rstd_b])

        def rms_stats(t):
            bank, bb = rms_sq(t)
            rms_sqrt(bank, bb)
            rms_recip()

        def rms_apply(t, gc0, dst, dst_bufs, dcols):
            for kc in range(8):
                stt(POOL if kc in POOL_KC else DVE, dst[:, kc, dcols], xT[:, kc, tsl(t)], colv[:, gc0 + kc:gc0 + kc + 1], rstd,
                    ALU.mult, ALU.mult, rd=[xb[kc][t], rstd_b, colv_b], wr=[dst_bufs[kc]])

        pre0 = {"have": False}

        def rmsnorm(t, gc0, dst, dst_bufs, dcols):
            rms_stats(t)
            rms_apply(t, gc0, dst, dst_bufs, dcols)

        xio_s = [B.new_sem(f"dxio{t}") for t in range(NT)]

        def load_x(p):
            for t in range(NT):
                src = x_d[p, :, tsl(t)].rearrange("(k p) n -> p k n", p=128)
                B.dma(SP, xio_s[t], xT[:, :, tsl(t)], src, wr=[xb[k][t] for k in range(8)])

        def store_x(p, do_norm):
            for t in range(NT):
                if do_norm:
                    if t == 0 and pre0["have"]:
                        pre0["have"] = False
                    else:
                        rms_stats(t)
                    for kc in range(8):
                        stt(DVE, xT[:, kc, tsl(t)], xT[:, kc, tsl(t)], colv[:, 60 + kc:61 + kc], rstd,
                            ALU.mult, ALU.mult, rd=[rstd_b, colv_b], wr=[xb[kc][t]])
                dst = out_d[p, :, tsl(t)].rearrange("(k p) n -> p k n", p=128)
                B.dma(SP, xio_s[t], dst, xT[:, :, tsl(t)], rd=[xb[k][t] for k in range(8)])

        def ffn(l, f):
            gc0 = l * 30 + (0 if f == 0 else 16)
            lf = l * 2 + f
            nwd = {"n": 0}

            def wd_load(gi):
                j0, j1 = GROUPS[gi]
                s = gi % 2
                src = wd_d[lf, j0 * 128:j1 * 128, :].rearrange("(j p) n -> p j n", p=128)
                B.dma(POOL, wd_s[s], wd_slots[s][:, 0:j1 - j0, :], src, wr=[wd_b[s]])

            wd_load(0)
            wd_load(1)
            if pre0["have"]:
                pre0["have"] = False
            else:
                rms_stats(0)
            rms_apply(0, gc0, hT, [hb[k][0] for k in range(8)], tsl(0))
            sgi = 0

            def gu(j, jj, t, slot, slot_b):
                nonlocal sgi
                wv = slot.rearrange("p (k g c) -> p k g c", k=8, g=2)
                bg_, bgb = ps_next()
                for kc in range(8):
                    mm(bg_, wv[:, kc, 0, :], hT[:, kc, tsl(t)], kc == 0, kc == 7,
                       rd=[slot_b, hb[kc][t]], wr=[bgb], inc=(kc == 7))
                bu_, bub = ps_next()
                for kc in range(8):
                    mm(bu_, wv[:, kc, 1, :], hT[:, kc, tsl(t)], kc == 0, kc == 7,
                       rd=[slot_b, hb[kc][t]], wr=[bub], inc=(kc == 7))
                si = sgi % 3
                sgi += 1
                act(sg[si], bg_, AF.Silu, rd=[bgb], wr=[sg_b[si]])
                tt(DVE, aT[:, jj, tsl(t)], sg[si], bu_, ALU.mult, rd=[sg_b[si], bub], wr=[ab[jj][t]])

            for gi, (j0, j1) in enumerate(GROUPS):
                if gi >= DBG["ng"]:
                    return
                jstart = j0
                if gi == 0:
                    NI = 3
                    slots = [ringA_acquire(("gu", l, f, j0 + k), ahead=k) for k in range(NI)]
                    for t in range(NT):
                        if t < NT - 1:
                            rmsnorm(t + 1, gc0, hT, [hb[k][t + 1] for k in range(8)], tsl(t + 1))
                        for k in range(NI):
                            gu(j0 + k, k, t, *slots[k])
                    for k in range(NI):
                        ringA_release()
                    jstart = j0 + NI
                for j in range(jstart, j1):
                    jj = j - j0
                    slot, slot_b = ringA_acquire(("gu", l, f, j))
                    for t in range(NT):
                        gu(j, jj, t, slot, slot_b)
                    ringA_release()
                s = gi % 2
                if DBG["ffn"] == 2:
                    continue
                for t in range(NT):
                    for m in range(8):
                        by_, byb = ps_next()
                        n = j1 - j0
                        for jj in range(n):
                            mm(by_, wd_slots[s][:, jj, m * 128:(m + 1) * 128], aT[:, jj, tsl(t)], jj == 0, jj == n - 1,
                               rd=[wd_b[s], ab[jj][t]], wr=[byb], inc=(jj == n - 1))
                        stt(DVE, xT[:, m, tsl(t)], by_, 0.5, xT[:, m, tsl(t)], ALU.mult, ALU.add,
                            rd=[byb], wr=[xb[m][t]])
                    if gi == len(GROUPS) - 1 and t == 0 and DBG["ng"] == 4:
                        rms_stats(0)
                        pre0["have"] = True
                if gi + 2 < len(GROUPS):
                    wd_load(gi + 2)

        GAMMA = [1.0 - 2.0 ** (-5.0 - h) for h in range(4)]
        GCH = [g ** CH for g in GAMMA]

        def mixer(l):
            XE = POOL if OFFLOAD else DVE
            gc0 = l * 30 + 8
            cw0 = l * 30 + 24
            X = mybir.AxisListType.X
            B.dma(SP, rowb_s, av(oD + 14336, 1024, F32), rowb_d[l, :, :], wr=[rowb_b])
            B.dma(SP, rowb_s, wsT_f, wst_d[l, :, :], wr=[wst_b])
            rowb_b.w = wst_b.w = (rowb_s, rowb_s.cnt)
            tt(DVE, wsT_b, wsT_f.rearrange("p (g n) -> p g n", g=4),
               maskT.unsqueeze(1).to_broadcast([128, 4, 128]), ALU.mult, rd=[wst_b, cst_b], wr=[wsTb_b])
            for i in range(4):
                B.dma(POOL, winc_s[i], winc_flat[i], winc_d[l * 4 + i, :, :], wr=[winc_b[i]])
            B.dma(POOL, wva_s, wva_flat, wins_d[l * 5 + 1, :, :], wr=[wva_b])
            B.op(DVE, lambda e: e.memset(vz.rearrange("p g n -> p (g n)"), 0.0), wr=[vz_b])
            for p_ in range(2):
                B.op(DVE, lambda e, o=var8[p_]: e.memset(o, 1.0), wr=[var8_b[p_]])
            st = {}

            t1q = t1.rearrange("p (h t i) -> p h t i", h=4, t=2)
            t2q = t2.rearrange("p (h t i) -> p h t i", h=4, t=2)

            def rotary(src, srcb, dst, dstb, c):
                sv_ = src.rearrange("p (h t i) -> p h t i", h=4, t=2)
                cos4 = cosT[:, c, :].unsqueeze(1).unsqueeze(1).to_broadcast([128, 4, 2, 64])
                sinb = sinT[:, c, :].unsqueeze(1).to_broadcast([128, 4, 64])
                tt(DVE, t1q, sv_, cos4, ALU.mult, rd=[srcb, cs_b], wr=[t1_b])
                stt(DVE, t2q[:, :, 0, :], sv_[:, :, 1, :], -1.0, sinb, ALU.mult, ALU.mult, rd=[srcb, cs_b], wr=[t2_b])
                tt(DVE, t2q[:, :, 1, :], sv_[:, :, 0, :], sinb, ALU.mult, rd=[srcb, cs_b], wr=[t2_b])
                tt(DVE, dst, t1, t2, ALU.add, rd=[t1_b, t2_b], wr=[dstb])

            def PRE_apply(t):
                par = t % 2
                rms_recip()
                rms_apply(t, gc0, hm2[par], [hm_b2[par]] * 8, slice(0, TILE))

            def PRE_fm(t):
                par = t % 2
                hm, hm_b = hm2[par], hm_b2[par]
                fm = {}
                for nm in ("u", "bg", "cg", "xin"):
                    slot, slot_b = ringA_acquire((nm, l, t))
                    wv = slot.rearrange("p (k c) -> p k c", k=8)
                    for ch in range(2):
                        bk, bkb = ps_next()
                        for kc in range(8):
                            mm(bk, wv[:, kc, ch * 128:(ch + 1) * 128], hm[:, kc, :], kc == 0, kc == 7,
                               rd=[slot_b, hm_b], wr=[bkb], inc=(kc == 7))
                        fm[(nm, ch)] = (bk, bkb)
                        if nm == "u":
                            act(uT[par][:, ch, :], bk, AF.Gelu_apprx_tanh, rd=[bkb], wr=[uT_b[par]])
                    ringA_release()
                for ch in range(2):
                    bgk, bgb_ = fm[("bg", ch)]
                    cgk, cgb_ = fm[("cg", ch)]
                    xik, xib_ = fm[("xin", ch)]
                    B.op(ACT, lambda e, i=cgk: e.copy(out=t1, in_=i), rd=[cgb_], wr=[t1_b])
                    if t == 0:
                        B.op(DVE, lambda e, o=zc[:, ch, 0:2]: e.memset(o, 0.0), wr=[zc_b[ch]])
                    else:
                        B.op(DVE, lambda e, o=zc[:, ch, 0:2], i=zc[:, ch, 512:514]: e.tensor_copy(out=o, in_=i),
                             rd=[zc_b[ch]], wr=[zc_b[ch]])
                    tt(DVE, zc[:, ch, 2:514], t1, xik, ALU.mult, rd=[t1_b, xib_], wr=[zc_b[ch]])
                    c0 = cw0 + ch * 3
                    ts(DVE, t2, zc[:, ch, 2:514], colv[:, c0 + 2:c0 + 3], None, ALU.mult, None,
                       rd=[zc_b[ch], colv_b], wr=[t2_b])
                    stt(DVE, t2, zc[:, ch, 1:513], colv[:, c0 + 1:c0 + 2], t2, ALU.mult, ALU.add,
                        rd=[zc_b[ch], t2_b], wr=[t2_b])
                    stt(DVE, t2, zc[:, ch, 0:512], colv[:, c0:c0 + 1], t2, ALU.mult, ALU.add,
                        rd=[zc_b[ch], t2_b], wr=[t2_b])
                    tt(DVE, ycat[par][:, 2 + ch, :], t2, bgk, ALU.mult, rd=[t2_b, bgb_], wr=[ycB_b[par]])

            def OUT(t):
                par = t % 2
                rdy = ycA_b[par] + [ycB_b[par]] + ycC_b[par]
                for wo in range(4):
                    slot, slot_b = ringA_acquire((f"wo{wo}", l, t))
                    wv = slot.rearrange("p (k c) -> p k c", k=8)
                    for mh in range(2):
                        m = wo * 2 + mh
                        bo, bob = ps_next()
                        for kc in range(8):
                            mm(bo, wv[:, kc, mh * 128:(mh + 1) * 128], ycat[par][:, kc, :], kc == 0, kc == 7,
                               rd=[slot_b] + rdy, wr=[bob], inc=(kc == 7))
                        tt(DVE, xT[:, m, tsl(t)], bo, xT[:, m, tsl(t)], ALU.add, rd=[bob], wr=[xb[m][t]])
                    ringA_release()

            def proj(c, i_or_va):
                cq = c % 4
                hm, hm_b = hm2[(c // 4) % 2], hm_b2[(c // 4) % 2]
                csl = slice(cq * 128, (cq + 1) * 128)
                bk, bkb = ps_next()
                if i_or_va == "va":
                    for kc in range(8):
                        mm(bk[:, 0:256], hm[:, kc, csl], wva[:, kc, :], kc == 0, kc == 7,
                           rd=[hm_b, wva_b], wr=[bkb], inc=(kc == 7))
                else:
                    i = i_or_va
                    for kc in range(8):
                        mm(bk, hm[:, kc, csl], winc[i][:, kc, :], kc == 0, kc == 7,
                           rd=[hm_b, winc_b[i]], wr=[bkb], inc=(kc == 7))
                return bk, bkb

            def blkB(c):
                pva, pva_b = proj(c, "va")
                pq_, pq_b = proj(c, 0)
                act(vg, pva[:, 0:256], AF.Gelu_apprx_tanh, rd=[pva_b], wr=[vg_b])
                rotary(pq_, pq_b, qrot, qrot_b, c)

            def blkD(c):
                par = c % 2
                pk_, pk_b = proj(c, 1)
                pv_, pv_b = proj(c, 2)
                B.op(ACT, lambda e, o=vtok[par], i=pv_: e.copy(out=o, in_=i), rd=[pv_b], wr=[vtok_b[par]])
                rotary(pk_, pk_b, krot, krot_b, c)
                tt(XE, kz[par].rearrange("p (h d) -> p h d", h=4), krot.rearrange("p (h d) -> p h d", h=4),
                   zs.unsqueeze(2).to_broadcast([128, 4, 128]), ALU.mult, rd=[krot_b, cst_b], wr=[kz_b[par]])

            def A_stats1(c):
                vg3 = vg.rearrange("p (g i) -> p g i", g=4)
                B.op(DVE, lambda e: e.reduce_sum(out=s1A, in_=vg3, axis=X), rd=[vg_b], wr=[s1A_b])
                stt(DVE, vg3, s1A.unsqueeze(2).to_broadcast([128, 4, 64]), -1.0 / 64, vg3, ALU.mult, ALU.add,
                    rd=[s1A_b, vg_b], wr=[vg_b])
                act(sqA, vg, AF.Square, rd=[vg_b], wr=[sqA_b], scale=0.125)

            def A_stats2(c, p8):
                B.op(DVE, lambda e, o=var8[p8][:, 0:4]: e.reduce_sum(out=o, in_=sqA.rearrange("p (g i) -> p g i", g=4), axis=X),
                     rd=[sqA_b], wr=[var8_b[p8]])

            def A_finish(c, p8):
                vg3 = vg.rearrange("p (g i) -> p g i", g=4)
                tt(XE, vg3, vg3, rs8[p8][:, 0:4].unsqueeze(2).to_broadcast([128, 4, 64]), ALU.mult,
                   rd=[vg_b, rs8_b[p8]], wr=[vg_b])
                vzp = vz.rearrange("p (a e) n -> p a e n", e=2)
                vgp = vg.rearrange("p (a e i) -> p a e i", a=2, e=2)
                vnp = vnorm_b.rearrange("p (a e i) -> p a e i", a=2, e=2)
                for e_ in range(2):
                    tt(XE, vzp[:, :, e_, e_ * 64:(e_ + 1) * 64], vgp[:, :, e_, :], vnp[:, :, e_, :], ALU.mult,
                       rd=[vg_b, rowb_b], wr=[vz_b])

            def blkF(c):
                par = c % 2
                pg_, pg_b = proj(c, 3)
                act(sgc[par], pg_, AF.Tanh, rd=[pg_b], wr=[sgc_b[par]], scale=0.5)
                st[("pg", c)] = (pg_, pg_b)

            def blkF2(c):
                par = c % 2
                pg_, pg_b = st[("pg", c)]
                stt(DVE, sgc[par], sgc[par], 1.0, pg_, ALU.add, ALU.mult, rd=[sgc_b[par], pg_b], wr=[sgc_b[par]])

            def blkA(c):
                bk, bkb = ps_next()
                bkv = bk.bitcast(BF16)
                for h in range(4):
                    tr(bkv[:, h * 128:(h + 1) * 128], qrot[:, h * 128:(h + 1) * 128], ident_b[:, :],
                       rd=[qrot_b, idb_b], wr=[bkb], inc=False)
                for h in range(4):
                    tr(bkv[:, 512 + h * 128:512 + (h + 1) * 128], krot[:, h * 128:(h + 1) * 128], ident_b[:, :],
                       rd=[krot_b, idb_b], wr=[bkb], inc=(h == 3))
                B.op(ACT, lambda e, i=bkv: e.copy(out=qkT, in_=i), rd=[bkb], wr=[qkT_b])

            def gmlp(c):
                t = c // 4
                cq = c % 4
                part = t % 2
                csl = slice(cq * 128, (cq + 1) * 128)
                psv, psv_b = ps_next()
                for g in range(4):
                    mm(psv[:, (g // 2) * 128:(g // 2 + 1) * 128], vz[:, g, :], wsT_b[:, g, :],
                       g % 2 == 0, g % 2 == 1, rd=[vz_b, wsTb_b], wr=[psv_b], inc=(g == 3))
                tt(DVE, svt.rearrange("p (k n) -> p k n", k=2), psv[:, 0:256].rearrange("p (k n) -> p k n", k=2),
                   bsb, ALU.add, rd=[psv_b, rowb_b], wr=[svt_b])
                tt(DVE, ycat[part][:, 0:2, csl], svt.rearrange("p (k n) -> p k n", k=2), uT[part][:, :, csl], ALU.mult,
                   rd=[svt_b, uT_b[part]], wr=[ycA_b[part][cq]])

            def blkC(c):
                pss, pss_b = ps_next()
                for h in range(4):
                    mm(pss[:, h * 128:(h + 1) * 128], qkT[:, 512 + h * 128:512 + (h + 1) * 128],
                       qkT[:, h * 128:(h + 1) * 128], True, True, rd=[qkT_b], wr=[pss_b], inc=(h == 3))
                tt(DVE, sT, pss.rearrange("p (h n) -> p h n", h=4), M2, ALU.mult, rd=[pss_b, cst_b], wr=[sT_b])

            def blkE(c):
                par = c % 2
                pr, pr_b = ps_next()
                for h in range(4):
                    o = pr[:, h * 128:(h + 1) * 128]
                    if c == 0:
                        mm(o, sT[:, h, :], vtok[par][:, h * 128:(h + 1) * 128], True, True,
                           rd=[sT_b, vtok_b[par]], wr=[pr_b], inc=(h == 3))
                    else:
                        mm(o, sT[:, h, :], vtok[par][:, h * 128:(h + 1) * 128], True, False,
                           rd=[sT_b, vtok_b[par]], wr=[pr_b], inc=False)
                        mm(o, qkT[:, h * 128:(h + 1) * 128], Sbf[:, h, :], False, True,
                           rd=[qkT_b, Sbf_b], wr=[pr_b], inc=(h == 3))
                st[c] = (pr, pr_b)
                if c < NCH - 1:
                    pkv, pkv_b = ps_next()
                    for h in range(4):
                        mm(pkv[:, h * 128:(h + 1) * 128], kz[par][:, h * 128:(h + 1) * 128],
                           vtok[par][:, h * 128:(h + 1) * 128], True, True,
                           rd=[kz_b[par], vtok_b[par]], wr=[pkv_b], inc=(h == 3))
                    pkv3 = pkv.rearrange("p (h n) -> p h n", h=4)
                    if c == 0:
                        B.op(DVE, lambda e, i=pkv3: e.tensor_copy(out=Sst, in_=i), rd=[pkv_b], wr=[S_b])
                    else:
                        tt(DVE, Sst, Sst, pkv3, ALU.add, rd=[S_b, pkv_b], wr=[S_b])
                    B.op(ACT, lambda e: e.copy(out=Sbf, in_=Sst), rd=[S_b], wr=[Sbf_b])
                    if c < NCH - 2:
                        tt(XE, Sst, Sst, gch_c.unsqueeze(2).to_broadcast([128, 4, 128]), ALU.mult,
                           rd=[S_b, cst_b], wr=[S_b])

            def C_stats1(c):
                pr, pr_b = st[c]
                pr3 = pr.rearrange("p (h n) -> p h n", h=4)
                rn3 = rn.rearrange("p (h n) -> p h n", h=4)
                B.op(DVE, lambda e, i=pr3: e.reduce_sum(out=s1C, in_=i, axis=X), rd=[pr_b], wr=[s1C_b])
                stt(DVE, rn3, s1C.unsqueeze(2).to_broadcast([128, 4, 128]), -1.0 / 128, pr3, ALU.mult, ALU.add,
                    rd=[s1C_b, pr_b], wr=[rn_b])
                act(sqC, rn, AF.Square, rd=[rn_b], wr=[sqC_b], scale=128.0 ** -0.5)

            def C_stats2(c, p8):
                B.op(DVE, lambda e, o=var8[p8][:, 4:8]: e.reduce_sum(out=o, in_=sqC.rearrange("p (h n) -> p h n", h=4), axis=X),
                     rd=[sqC_b], wr=[var8_b[p8]])

            def joint_a(p8):
                tt(DVE, var8[p8], var8[p8], eps8, ALU.add, rd=[var8_b[p8], cst_b], wr=[var8_b[p8]])
                act(rs8[p8], var8[p8], AF.Sqrt, rd=[var8_b[p8]], wr=[rs8_b[p8]])

            def joint_b(p8):
                B.op(DVE, lambda e, o=rs8[p8]: e.reciprocal(out=o, in_=o), rd=[rs8_b[p8]], wr=[rs8_b[p8]])
                ts(DVE, rs8[p8][:, 4:8], rs8[p8][:, 4:8], 0.5, None, ALU.mult, None, rd=[rs8_b[p8]], wr=[rs8_b[p8]])

            def C_finish(c, p8):
                par = c % 2
                rn3 = rn.rearrange("p (h n) -> p h n", h=4)
                tt(XE, rn3, rn3, rs8[p8][:, 4:8].unsqueeze(2).to_broadcast([128, 4, 128]), ALU.mult,
                   rd=[rn_b, rs8_b[p8]], wr=[rn_b])
                tt(XE, rn, rn, gn_b, ALU.mult, rd=[rn_b, rowb_b], wr=[rn_b])
                tt(XE, yc[par], rn, sgc[par], ALU.mult, rd=[rn_b, sgc_b[par]], wr=[yc_b[par]])

            def blkG(c):
                t = c // 4
                cq = c % 4
                part = t % 2
                par = c % 2
                csl = slice(cq * 128, (cq + 1) * 128)
                bk, bkb = ps_next()
                bkv = bk.bitcast(BF16)
                for h in range(4):
                    tr(bkv[:, h * 128:(h + 1) * 128], yc[par][:, h * 128:(h + 1) * 128], ident_b[:, :],
                       rd=[yc_b[par], idb_b], wr=[bkb], inc=(h == 3))
                B.op(ACT, lambda e, o=ycat[part][:, 4:8, csl], i=bkv[:, 0:512].rearrange("p (h n) -> p h n", h=4):
                     e.copy(out=o, in_=i), rd=[bkb], wr=[ycC_b[part][cq]])

            if pre0["have"]:
                pre0["have"] = False
            else:
                bank0, bb0 = rms_sq(0)
                rms_sqrt(bank0, bb0)
                rms_recip()
            rms_apply(0, gc0, hm2[0], [hm_b2[0]] * 8, slice(0, TILE))
            PRE_fm(0)
            for i in range(NCH + 2):
                p8 = i % 2
                cur = i < NCH
                prv = 0 <= i - 1 < NCH
                nrm = cur and i % 4 == 1 and i + 3 < NCH
                if cur:
                    blkB(i)
                if prv:
                    blkC(i - 1)
                if cur:
                    blkD(i)
                if nrm:
                    nbank, nbb = rms_sq((i + 3) // 4)
                if cur:
                    A_stats1(i)
                if prv:
                    blkE(i - 1)
                    C_stats1(i - 1)
                if cur:
                    A_stats2(i, p8)
                if prv:
                    gmlp(i - 1)
                if cur:
                    blkF(i)
                    blkA(i)
                if prv:
                    C_stats2(i - 1, p8)
                if cur or prv:
                    joint_a(p8)
                if nrm:
                    rms_sqrt(nbank, nbb)
                if cur:
                    blkF2(i)
                if cur or prv:
                    joint_b(p8)
                if cur:
                    A_finish(i, p8)
                if prv:
                    C_finish(i - 1, p8)
                if nrm:
                    PRE_apply((i + 3) // 4)
                if 0 <= i - 2 < NCH:
                    blkG(i - 2)
                    if (i - 2) % 4 == 3:
                        OUT((i - 2) // 4)
                if cur and i % 4 == 2 and i + 2 < NCH:
                    PRE_fm((i + 2) // 4)
                if i == 11:
                    rms_stats(0)
                    pre0["have"] = True

        cur_B, cur_C, cur_D = [], [], []

        def set_regions(newB, newC, newD=None):
            nonlocal cur_B, cur_C, cur_D
            if newB is not cur_B:
                alias_switch(cur_B, newB)
                cur_B = newB
            if newC is not cur_C:
                alias_switch(cur_C, newC)
                cur_C = newC
            if newD is not None and newD is not cur_D:
                alias_switch(cur_D, newD)
                cur_D = newD

        for p in range(n_seq):
            load_x(p)
            for (_, l, kind, f) in run_phases[p]:
                if kind == "ffn":
                    set_regions(regB_ffn, regC_ffn, regD_ffn)
                    ffn(l, f)
                else:
                    set_regions(regB_mix, regC_mix, regD_mix)
                    mixer(l)
            store_x(p, final_norm and n_phases is None)
            pre0["have"] = False
        B.op(SP, lambda e: e.nop(), wr=[xb[k][t] for k in range(8) for t in range(NT)])

        with nc.Block() as block:
            @block.tensor
            def _(e):
                _replay(e, PE.ops)

            @block.scalar
            def _(e):
                _replay(e, ACT.ops)

            @block.vector
            def _(e):
                _replay(e, DVE.ops)

            @block.gpsimd
            def _(e):
                _replay(e, POOL.ops)

            @block.sync
            def _(e):
                _replay(e, SP.ops)
        stats = {k.name: len(k.ops) for k in (PE, ACT, DVE, POOL, SP)}
        nc._k_stats = stats
    return nc


def _consts():
    idx = np.arange(CH, dtype=np.float64)
    gam = np.array([1.0 - 2.0 ** (-5.0 - h) for h in range(4)], dtype=np.float64)
    logg = np.log(gam)
    scale = 128.0 ** -0.5
    cst = np.zeros((128, NCST), np.float64)
    cst[:, 0:128] = np.eye(128)
    s = idx[:, None]
    t = idx[None, :]
    for h in range(4):
        cst[:, 128 + h * 128:128 + (h + 1) * 128] = (s <= t) * np.exp(-logg[h] * (s + 1.0)) * scale
    cst[:, 640:768] = (s <= t)
    for h in range(4):
        cst[:, 768 + h] = np.exp(logg[h] * (CH - 1 - idx)) * scale
        cst[:, 772 + h] = EPS / np.exp(logg[h] * (idx + 1.0)) ** 2
        cst[:, 776 + h] = gam[h] ** CH
        cst[:, 780 + h] = EPS
        cst[:, 784 + h] = EPS / np.exp(logg[h] * (idx + 1.0)) ** 2
    half = 64
    inv = (10000.0 ** (-np.arange(half, dtype=np.float32) / half)).astype(np.float32)
    pos = np.arange(SEQ, dtype=np.float32)
    ang = (pos[:, None] * inv[None, :]).astype(np.float32)
    cos = np.cos(ang.astype(np.float64)).reshape(NCH, 128, half).transpose(1, 0, 2).reshape(128, NCH * half)
    sin = np.sin(ang.astype(np.float64)).reshape(NCH, 128, half).transpose(1, 0, 2).reshape(128, NCH * half)
    cs = np.concatenate([cos, sin], axis=1)
    return cst.astype(np.float32), cs.astype(np.float32)


def _prep_weights(inp):
    f = lambda a: np.ascontiguousarray(np.asarray(a, dtype=np.float32))
    wg = [f(inp["ffn1_w_gate"]), f(inp["ffn2_w_gate"])]
    wu = [f(inp["ffn1_w_up"]), f(inp["ffn2_w_up"])]
    wdn = [f(inp["ffn1_w_down"]), f(inp["ffn2_w_down"])]
    wgu = np.empty((L, 2, NJ, 128, 8, 2, 128), np.float32)
    wd = np.empty((L, 2, DFF, D), np.float32)
    for l in range(L):
        for ff in range(2):
            g = wg[ff][l].reshape(8, 128, NJ, 128).transpose(2, 1, 0, 3)
            u = wu[ff][l].reshape(8, 128, NJ, 128).transpose(2, 1, 0, 3)
            wgu[l, ff, :, :, :, 0, :] = g
            wgu[l, ff, :, :, :, 1, :] = u
            wd[l, ff] = wdn[ff][l]
    w_in = f(inp["w_in"])
    wins = np.empty((L, 5, 128, 8, 256), np.float32)
    winc = np.empty((L, 4, 128, 8, 512), np.float32)
    for l in range(L):
        for i in range(5):
            wins[l, i] = w_in[l][:, i * 256:(i + 1) * 256].reshape(8, 128, 256).transpose(1, 0, 2)
        for i in range(4):
            winc[l, i] = w_in[l][:, 1280 + i * 512:1280 + (i + 1) * 512].reshape(8, 128, 512).transpose(1, 0, 2)
    w_out = f(inp["w_out"])
    wout = np.empty((L, 4, 128, 8, 256), np.float32)
    for l in range(L):
        for i in range(4):
            wout[l, i] = w_out[l][:, i * 256:(i + 1) * 256].reshape(8, 128, 256).transpose(1, 0, 2)
    colv = np.zeros((128, NCV), np.float32)
    for l in range(L):
        colv[:, l * 30 + 0:l * 30 + 8] = f(inp["ffn1_norm"])[l].reshape(8, 128).T
        colv[:, l * 30 + 8:l * 30 + 16] = f(inp["mix_norm"])[l].reshape(8, 128).T
        colv[:, l * 30 + 16:l * 30 + 24] = f(inp["ffn2_norm"])[l].reshape(8, 128).T
        cw = f(inp["conv_w"])[l]
        for ch in range(2):
            for i in range(3):
                colv[:, l * 30 + 24 + ch * 3 + i] = cw[i, ch * 128:(ch + 1) * 128]
    colv[:, 60:68] = f(inp["final_norm"]).reshape(8, 128).T
    rowb = np.empty((L, 128, 1024), np.float32)
    bs = f(inp["gmlp_b_s"])
    for l in range(L):
        rowb[l, :, 0:256] = f(inp["gmlp_v_norm"])[l][None, :]
        rowb[l, :, 256:768] = f(inp["ret_gn"])[l][None, :]
        for ch in range(2):
            rowb[l, 0:64, 768 + ch * 128:768 + (ch + 1) * 128] = bs[l, 2 * ch][None, :]
            rowb[l, 64:128, 768 + ch * 128:768 + (ch + 1) * 128] = bs[l, 2 * ch + 1][None, :]
    wst = np.ascontiguousarray(f(inp["gmlp_w_s"]).transpose(0, 3, 1, 2)).reshape(L, 128, 512)
    cst, cs = _consts()
    return {
        "wgu": wgu.reshape(L * 2 * NJ, 128, 2048),
        "wd": wd.reshape(L * 2, DFF, D),
        "wins": wins.reshape(L * 5, 128, 2048),
        "winc": winc.reshape(L * 4, 128, 4096),
        "wout": wout.reshape(L * 4, 128, 2048),
        "colv": colv, "rowb": rowb, "wst": wst, "cst": cst, "cossin": cs,
    }


_NC_CACHE = {}


def kernel(**inputs):
    return _run(inputs)


def _run(inputs, n_phases=None, final_norm=True, n_cores=8):
    x = np.ascontiguousarray(np.asarray(inputs["x"], dtype=np.float32))
    shared = _prep_weights(inputs)
    key = (n_phases, final_norm)
    if key not in _NC_CACHE:
        _NC_CACHE[key] = build(n_phases, final_norm)
    nc = _NC_CACHE[key]
    n = n_cores
    in_maps = []
    for c in range(n):
        m = dict(shared)
        m["x"] = np.ascontiguousarray(x[c * NSEQ:(c + 1) * NSEQ].transpose(0, 2, 1))
        in_maps.append(m)
    res = run_bass_kernel_spmd(nc, in_maps, core_ids=list(range(n)))
    out = np.concatenate([np.asarray(r["out"]).transpose(0, 2, 1) for r in res.results], axis=0)
    out = np.ascontiguousarray(out)
    return out.astype(np.float32, copy=False)
```
